# Optimizing a Trainium2 kernel written in Bass

```python
import math
import jax, jax.numpy as jnp
from jax import lax
import numpy as np

D_MODEL = 1024
BATCH = 8
SEQ = 4096
DEPTH = 2

N_MIXERS = 2
DN_HEADS = 8
DN_HEAD_DIM = 128
DN_DIM = DN_HEADS * DN_HEAD_DIM
DN_CONV = 4
DN_CHUNK = 64
DN_IN = 4 * DN_DIM + 2 * DN_HEADS
MB_HEADS = 8
MB_HEAD_DIM = D_MODEL // MB_HEADS
MB_DIM = MB_HEADS * MB_HEAD_DIM
MB_BLOCK = 256
MB_TOPK = 3
MB_QCHUNK = 16
D_FF = 4 * D_MODEL
RMS_EPS = 1e-6
N_DN_LAYERS = (DEPTH + 1) // 2
N_MB_LAYERS = DEPTH // 2

kernel_name = "hybrid_gdn_moba_alibi_sqrelu"


def rmsnorm(x, g):
    xf = x.astype(jnp.float32)
    y = xf * lax.rsqrt(jnp.mean(xf * xf, axis=-1, keepdims=True) + RMS_EPS)
    return (y * g.astype(jnp.float32)).astype(x.dtype)


def l2norm(x):
    xf = x.astype(jnp.float32)
    return xf * lax.rsqrt(jnp.sum(xf * xf, axis=-1, keepdims=True) + 1e-6)


def causal_depthwise_conv(x, w):
    k_w = w.shape[0]
    t = x.shape[1]
    xp = jnp.pad(x, ((0, 0), (k_w - 1, 0), (0, 0)))
    y = xp[:, 0:t] * w[0]
    for j in range(1, k_w):
        y = y + xp[:, j:j + t] * w[j]
    return y


def alibi_slopes(n_heads):
    return jnp.exp2(-8.0 * jnp.arange(1, n_heads + 1, dtype=jnp.float32) / n_heads)


def chunk_gated_delta_rule(q, k, v, g, beta):
    b, h, t, dk = q.shape
    dv = v.shape[-1]
    c = DN_CHUNK
    n = t // c
    q = q.astype(jnp.float32).reshape(b, h, n, c, dk)
    k = k.astype(jnp.float32).reshape(b, h, n, c, dk)
    v = v.astype(jnp.float32).reshape(b, h, n, c, dv)
    g = g.astype(jnp.float32).reshape(b, h, n, c)
    beta = beta.astype(jnp.float32).reshape(b, h, n, c)

    gc = jnp.cumsum(g, axis=-1)
    idx = jnp.arange(c)
    tri_incl = idx[:, None] >= idx[None, :]
    tri_strict = idx[:, None] > idx[None, :]
    decay = jnp.exp(jnp.where(tri_incl, gc[..., :, None] - gc[..., None, :], -jnp.inf))

    k_beta = k * beta[..., None]
    a_low = jnp.where(tri_strict, jnp.einsum('bhncd,bhnsd->bhncs', k_beta, k) * decay, 0.0)
    a_mat = a_low + jnp.eye(c, dtype=jnp.float32)
    rhs = jnp.concatenate([v * beta[..., None], k_beta * jnp.exp(gc)[..., None]], axis=-1)
    sol = lax.linalg.triangular_solve(a_mat, rhs, left_side=True, lower=True, unit_diagonal=True)
    u, w = sol[..., :dv], sol[..., dv:]

    attn_intra = jnp.einsum('bhncd,bhnsd->bhncs', q, k) * decay
    q_dec = q * jnp.exp(gc)[..., None]
    k_dec = k * jnp.exp(gc[..., -1:] - gc)[..., None]
    g_tot = jnp.exp(gc[..., -1])

    xs = tuple(jnp.moveaxis(a, 2, 0) for a in (u, w, q_dec, k_dec, attn_intra, g_tot))

    def step(state, inp):
        u_c, w_c, qd_c, kd_c, at_c, gt_c = inp
        v_new = u_c - jnp.einsum('bhcd,bhde->bhce', w_c, state)
        o_c = jnp.einsum('bhcd,bhde->bhce', qd_c, state) + jnp.einsum('bhcs,bhse->bhce', at_c, v_new)
        state = state * gt_c[..., None, None] + jnp.einsum('bhcd,bhce->bhde', kd_c, v_new)
        return state, o_c

    s0 = jnp.zeros((b, h, dk, dv), jnp.float32)
    _, o = lax.scan(step, s0, xs)
    return jnp.moveaxis(o, 0, 2).reshape(b, h, t, dv)


def gated_deltanet(hn, w_in, conv_w, a_log, dt_bias, out_norm_g, w_out):
    b, t, _ = hn.shape
    proj = hn @ w_in
    qkv = jax.nn.silu(causal_depthwise_conv(proj[..., :3 * DN_DIM], conv_w))
    z = proj[..., 3 * DN_DIM:4 * DN_DIM]
    b_raw = proj[..., 4 * DN_DIM:4 * DN_DIM + DN_HEADS].astype(jnp.float32)
    a_raw = proj[..., 4 * DN_DIM + DN_HEADS:].astype(jnp.float32)

    def heads(a):
        return a.reshape(b, t, DN_HEADS, DN_HEAD_DIM).transpose(0, 2, 1, 3)

    q = heads(l2norm(heads(qkv[..., :DN_DIM]).transpose(0, 2, 1, 3)) * DN_HEAD_DIM ** -0.5)
    k = heads(l2norm(heads(qkv[..., DN_DIM:2 * DN_DIM]).transpose(0, 2, 1, 3)))
    v = heads(qkv[..., 2 * DN_DIM:])
    beta = jax.nn.sigmoid(b_raw).transpose(0, 2, 1)
    g = (-jnp.exp(a_log.astype(jnp.float32)) *
         jax.nn.softplus(a_raw + dt_bias.astype(jnp.float32))).transpose(0, 2, 1)
    o = chunk_gated_delta_rule(q, k, v, g, beta)
    o = o.transpose(0, 2, 1, 3).astype(hn.dtype)
    o = rmsnorm(o, out_norm_g) * jax.nn.silu(z.reshape(b, t, DN_HEADS, DN_HEAD_DIM))
    return o.reshape(b, t, DN_DIM) @ w_out


def moba_attention(q, k, v):
    b, h, t, dh = q.shape
    nb = -(-t // MB_BLOCK)
    tp = nb * MB_BLOCK
    pad = ((0, 0), (0, 0), (0, tp - t), (0, 0))
    q, k, v = jnp.pad(q, pad), jnp.pad(k, pad), jnp.pad(v, pad)
    kb = k.reshape(b, h, nb, MB_BLOCK, dh)
    vb = v.reshape(b, h, nb, MB_BLOCK, dh)
    top = min(MB_TOPK, nb)
    scale = dh ** -0.5
    slopes = alibi_slopes(h)

    k_mean = jnp.mean(kb.astype(jnp.float32), axis=3)
    gate = jnp.einsum('bhtd,bhnd->bhtn', q.astype(jnp.float32), k_mean)
    q_blk = jnp.arange(tp) // MB_BLOCK
    past = jnp.arange(nb)[None, :] < q_blk[:, None]
    gate = jnp.where(past, gate, -jnp.inf)
    _, sel = lax.top_k(gate, top)
    valid = sel < q_blk[:, None]

    n_qc = tp // MB_QCHUNK

    def to_chunks(a):
        return jnp.moveaxis(a.reshape(b, h, n_qc, MB_QCHUNK, *a.shape[3:]), 2, 0)

    gather_blocks = jax.vmap(jax.vmap(lambda blocks, ids: blocks[ids]))

    def attend_chunk(args):
        qc, selc, validc, ci = args
        t0 = ci * MB_QCHUNK
        blk = t0 // MB_BLOCK
        tpos = (t0 + jnp.arange(MB_QCHUNK)).astype(jnp.float32)
        ks = gather_blocks(kb, selc)
        vs = gather_blocks(vb, selc)
        s_sel = jnp.einsum('bhqd,bhqnld->bhqnl', qc, ks).astype(jnp.float32) * scale
        kpos_sel = (selc[..., None] * MB_BLOCK + jnp.arange(MB_BLOCK)).astype(jnp.float32)
        dist_sel = tpos[:, None, None] - kpos_sel
        s_sel = jnp.where(validc[..., None], s_sel - slopes[:, None, None, None] * dist_sel, -jnp.inf)
        ko = lax.dynamic_index_in_dim(kb, blk, axis=2, keepdims=False)
        vo = lax.dynamic_index_in_dim(vb, blk, axis=2, keepdims=False)
        s_own = jnp.einsum('bhqd,bhld->bhql', qc, ko).astype(jnp.float32) * scale
        dist_own = tpos[:, None] - (blk * MB_BLOCK + jnp.arange(MB_BLOCK)).astype(jnp.float32)
        s_own = jnp.where(dist_own >= 0, s_own - slopes[:, None, None] * dist_own, -jnp.inf)
        logits = jnp.concatenate([s_sel.reshape(b, h, MB_QCHUNK, top * MB_BLOCK), s_own], axis=-1)
        p = jax.nn.softmax(logits, axis=-1).astype(vs.dtype)
        p_sel = p[..., :top * MB_BLOCK].reshape(b, h, MB_QCHUNK, top, MB_BLOCK)
        p_own = p[..., top * MB_BLOCK:]
        return (jnp.einsum('bhqnl,bhqnld->bhqd', p_sel, vs) +
                jnp.einsum('bhql,bhld->bhqd', p_own, vo))

    out = lax.map(attend_chunk, (to_chunks(q), to_chunks(sel), to_chunks(valid), jnp.arange(n_qc)))
    out = jnp.moveaxis(out, 0, 2).reshape(b, h, tp, dh)
    return out[:, :, :t]


def moba_layer(hn, w_in, w_out):
    b, t, _ = hn.shape
    proj = hn @ w_in

    def heads(a):
        return a.reshape(b, t, MB_HEADS, MB_HEAD_DIM).transpose(0, 2, 1, 3)

    q = heads(proj[..., :MB_DIM])
    k = heads(proj[..., MB_DIM:2 * MB_DIM])
    v = heads(proj[..., 2 * MB_DIM:])
    o = moba_attention(q, k, v)
    return o.transpose(0, 2, 1, 3).reshape(b, t, MB_DIM) @ w_out


def squared_relu_mlp(hn, w_up, w_down):
    return jnp.square(jax.nn.relu(hn @ w_up)) @ w_down


def setup_inputs(seed: int = 0) -> dict:
    key = jax.random.key(seed)
    ks = jax.random.split(key, 16)
    f32 = jnp.float32
    x = jax.random.normal(ks[0], (BATCH, SEQ, D_MODEL), f32)
    mix_norm_g = 1.0 + 0.02 * jax.random.normal(ks[1], (DEPTH, D_MODEL), f32)
    mlp_norm_g = 1.0 + 0.02 * jax.random.normal(ks[2], (DEPTH, D_MODEL), f32)
    dn_w_in = jax.random.normal(ks[3], (N_DN_LAYERS, D_MODEL, DN_IN), f32) * D_MODEL ** -0.5
    dn_conv_w = jax.random.normal(ks[4], (N_DN_LAYERS, DN_CONV, 3 * DN_DIM), f32) * DN_CONV ** -0.5
    dn_a_log = jnp.log(jax.random.uniform(ks[5], (N_DN_LAYERS, DN_HEADS), f32, 1.0, 16.0))
    dt = jnp.exp(jax.random.uniform(ks[6], (N_DN_LAYERS, DN_HEADS), f32, math.log(1e-3), math.log(1e-1)))
    dn_dt_bias = dt + jnp.log(-jnp.expm1(-dt))
    dn_out_norm_g = 1.0 + 0.02 * jax.random.normal(ks[7], (N_DN_LAYERS, DN_HEAD_DIM), f32)
    dn_w_out = jax.random.normal(ks[8], (N_DN_LAYERS, DN_DIM, D_MODEL), f32) * DN_DIM ** -0.5
    mb_w_in = jax.random.normal(ks[9], (N_MB_LAYERS, D_MODEL, 3 * MB_DIM), f32) * D_MODEL ** -0.5
    mb_w_out = jax.random.normal(ks[10], (N_MB_LAYERS, MB_DIM, D_MODEL), f32) * MB_DIM ** -0.5
    mlp_w_up = jax.random.normal(ks[11], (DEPTH, D_MODEL, D_FF), f32) * D_MODEL ** -0.5
    mlp_w_down = jax.random.normal(ks[12], (DEPTH, D_FF, D_MODEL), f32) * D_FF ** -0.5
    final_norm_g = 1.0 + 0.02 * jax.random.normal(ks[13], (D_MODEL,), f32)
    return {"x": x, "mix_norm_g": mix_norm_g, "mlp_norm_g": mlp_norm_g,
            "dn_w_in": dn_w_in, "dn_conv_w": dn_conv_w, "dn_a_log": dn_a_log,
            "dn_dt_bias": dn_dt_bias, "dn_out_norm_g": dn_out_norm_g, "dn_w_out": dn_w_out,
            "mb_w_in": mb_w_in, "mb_w_out": mb_w_out,
            "mlp_w_up": mlp_w_up, "mlp_w_down": mlp_w_down, "final_norm_g": final_norm_g}


def reference(x, mix_norm_g, mlp_norm_g, dn_w_in, dn_conv_w, dn_a_log, dn_dt_bias,
              dn_out_norm_g, dn_w_out, mb_w_in, mb_w_out, mlp_w_up, mlp_w_down, final_norm_g):
    h = x
    for i in range(DEPTH):
        hn = rmsnorm(h, mix_norm_g[i])
        j = i // N_MIXERS
        if i % N_MIXERS == 0:
            mix = gated_deltanet(hn, dn_w_in[j], dn_conv_w[j], dn_a_log[j], dn_dt_bias[j],
                                 dn_out_norm_g[j], dn_w_out[j])
        else:
            mix = moba_layer(hn, mb_w_in[j], mb_w_out[j])
        h = h + mix.astype(h.dtype)
        h = h + squared_relu_mlp(rmsnorm(h, mlp_norm_g[i]), mlp_w_up[i], mlp_w_down[i])
    return rmsnorm(h, final_norm_g)
```

```python
import numpy as np
from contextlib import ExitStack
import concourse.bass as bass
import concourse.mybir as mybir
from concourse.bass_utils import run_bass_kernel_spmd

F32 = mybir.dt.float32
BF16 = mybir.dt.bfloat16
AF = mybir.ActivationFunctionType
ALU = mybir.AluOpType
AX = mybir.AxisListType

T = 4096
D = 1024
NT = 32
H = 8
NEG = -30000.0
EPS = 1e-6

C_ID, C_MU, C_MSU, C_TRI, C_ONE, C_ALI, C_PAST = 0, 128, 256, 384, 512, 640, 896
NCST = 896 + 256


class Phase:
    def __init__(self, nc, name):
        self.nc = nc
        self.name = name
        self.ops = []

    def op(self, eng, fn, r=(), w=(), dma=False, key=None):
        self.ops.append(dict(eng=eng, fn=fn, reads=tuple(r), writes=tuple(w), dma=dma, key=key, signal=False))

    def mm(self, out, lhsT, rhs, start=True, stop=True, r=(), w=()):
        self.op('pe', lambda e: e.matmul(out, lhsT=lhsT, rhs=rhs, start=start, stop=stop), r, w)

    def tr(self, out, in_, ident, r=(), w=()):
        self.op('pe', lambda e: e.transpose(out=out, in_=in_, identity=ident), r, w)

    def act(self, out, in_, func, r=(), w=(), bias=None, scale=None, accum=None):
        kw = {}
        if bias is not None:
            kw['bias'] = bias
        if scale is not None:
            kw['scale'] = scale
        if accum is not None:
            kw['accum_out'] = accum
        self.op('act', lambda e: e.activation(out=out, in_=in_, func=func, **kw), r, w)

    def ts(self, eng, out, in0, s1, s2, op0, op1=None, r=(), w=()):
        if op1 is None:
            self.op(eng, lambda e: e.tensor_scalar(out=out, in0=in0, scalar1=s1, scalar2=None, op0=op0), r, w)
        else:
            self.op(eng, lambda e: e.tensor_scalar(out=out, in0=in0, scalar1=s1, scalar2=s2, op0=op0, op1=op1), r, w)

    def tt(self, eng, out, in0, in1, op, r=(), w=()):
        self.op(eng, lambda e: e.tensor_tensor(out=out, in0=in0, in1=in1, op=op), r, w)

    def stt(self, eng, out, in0, scalar, in1, op0, op1, r=(), w=()):
        self.op(eng, lambda e: e.scalar_tensor_tensor(out=out, in0=in0, scalar=scalar, in1=in1, op0=op0, op1=op1), r, w)

    def copy(self, eng, out, in_, r=(), w=()):
        if eng == 'act':
            self.op(eng, lambda e: e.copy(out=out, in_=in_), r, w)
        else:
            self.op(eng, lambda e: e.tensor_copy(out=out, in_=in_), r, w)

    def memset(self, eng, ap, val, w=()):
        self.op(eng, lambda e: e.memset(ap, val), (), w)

    def dma(self, eng, out, in_, r=(), w=(), key=None):
        self.op(eng, lambda e: e.dma_start(out=out, in_=in_), r, w, dma=True, key=key)

    def run(self):
        nc = self.nc
        ops = self.ops
        last_w = {}
        readers = {}
        deps = []
        for i, o in enumerate(ops):
            d = set()
            excl = [r for r in o['reads'] if r.startswith("PS:")]
            if excl:
                o['writes'] = tuple(o['writes']) + tuple(x for x in excl if x not in o['writes'])
            for r in o['reads']:
                if r in last_w:
                    d.add(last_w[r])
            for w in o['writes']:
                if w in last_w:
                    d.add(last_w[w])
                d.update(readers.get(w, ()))
            d.discard(i)
            for r in o['reads']:
                readers.setdefault(r, []).append(i)
            for w in o['writes']:
                last_w[w] = i
                readers[w] = []
            deps.append(d)
        eidx = []
        ecount = {}
        for o in ops:
            ecount[o['eng']] = ecount.get(o['eng'], 0) + 1
            eidx.append(ecount[o['eng']])

        def same_ok(i, d):
            o, od = ops[i], ops[d]
            if od['eng'] != o['eng'] or o['dma']:
                return False
            if o['eng'] == 'pe':
                return True
            return eidx[i] - eidx[d] > 3

        for i, o in enumerate(ops):
            for d in deps[i]:
                od = ops[d]
                if od['dma']:
                    continue
                if same_ok(i, d):
                    continue
                od['signal'] = True
        cnt = {}
        dcnt = {}
        for o in ops:
            if o['dma']:
                dcnt[o['key']] = dcnt.get(o['key'], 0) + 16
                o['dval'] = dcnt[o['key']]
            elif o['signal']:
                cnt[o['eng']] = cnt.get(o['eng'], 0) + 1
                o['cval'] = cnt[o['eng']]
        engs = ('pe', 'act', 'dve', 'pool', 'sp')
        with ExitStack() as es:
            esem = {e: nc.alloc_semaphore(name=f"{self.name}_s_{e}") for e in engs if cnt.get(e)}
            dsem = {k: nc.alloc_semaphore(name=f"{self.name}_d_{j}") for j, k in enumerate(dcnt)}
            seen = {e: {} for e in engs}
            for i, o in enumerate(ops):
                w = {}
                for d in deps[i]:
                    od = ops[d]
                    if od['dma']:
                        s, v = ('d', od['key']), od['dval']
                    elif same_ok(i, d):
                        continue
                    else:
                        s, v = ('e', od['eng']), od['cval']
                    if seen[o['eng']].get(s, 0) >= v:
                        continue
                    w[s] = max(w.get(s, 0), v)
                for s, v in w.items():
                    seen[o['eng']][s] = v
                o['waits'] = [((dsem[s[1]] if s[0] == 'd' else esem[s[1]]), v) for s, v in w.items()]
            fence = [(dsem[k], v) for k, v in dcnt.items()]
            block = es.enter_context(nc.Block())

            def make(ename):
                def body(eng):
                    for o in ops:
                        if o['eng'] != ename:
                            continue
                        for s, v in o['waits']:
                            eng.wait_ge(s, v)
                        ins = o['fn'](eng)
                        if o['dma']:
                            ins.then_inc(dsem[o['key']], 16)
                        elif o['signal']:
                            ins.then_inc(esem[ename], 1)
                    if ename == 'sp':
                        for s, v in fence:
                            eng.wait_ge(s, v)
                return body

            block.tensor(make('pe'))
            block.scalar(make('act'))
            block.vector(make('dve'))
            block.gpsimd(make('pool'))
            block.sync(make('sp'))
        nc.all_engine_barrier()
        nc.clear_and_free_semaphores(list(esem.values()) + list(dsem.values()))
        nc.all_engine_barrier()
        return len(ops)


class Rot:
    def __init__(self, items):
        self.items = items
        self.i = 0

    def next(self):
        r = self.items[self.i % len(self.items)]
        self.i += 1
        return r


_UID = [0]


def _sb(nc, es, name, shape, dt):
    _UID[0] += 1
    return es.enter_context(nc.sbuf_tensor(f"{name}_u{_UID[0]}", list(shape), dt))


def _ps(nc, es, name, shape, dt):
    _UID[0] += 1
    return es.enter_context(nc.psum_tensor(f"{name}_u{_UID[0]}", list(shape), dt))


def emit_norm_T(P, src, gamt, xin, hn, stat, junk, ptr, hnT_dst, identb, tag, evac_eng, hnT_res, keep=None):
    xt, xr = xin
    hnt, hr = hn
    stt_, sr = stat
    pt, pr = ptr
    P.dma('sp', xt[:], src, w=[xr], key=xr)
    P.act(junk[:], xt[:], AF.Square, accum=stt_[:, 0:1], r=[xr], w=[sr])
    P.ts('dve', stt_[:, 1:2], stt_[:, 0:1], 1.0 / D, EPS, ALU.mult, ALU.add, r=[sr], w=[sr])
    P.act(stt_[:, 2:3], stt_[:, 1:2], AF.Sqrt, r=[sr], w=[sr])
    P.op('dve', lambda e: e.reciprocal(out=stt_[:, 3:4], in_=stt_[:, 2:3]), [sr], [sr])
    P.stt('dve', hnt[:], xt[:], stt_[:, 3:4], gamt[:], ALU.mult, ALU.mult, r=[xr, sr, 'gam'], w=[hr])
    for c in range(8):
        P.tr(pt[:, c * 128:(c + 1) * 128], hnt[:, c * 128:(c + 1) * 128], identb[:], r=[hr, 'identb'], w=[pr])
    P.copy(evac_eng, hnT_dst, pt[:].rearrange("p (c t) -> p c t", c=8), r=[pr], w=[hnT_res])


def build(debug=False, upto=99):
    nc = bass.Bass("TRN2", target_bir_lowering=False)

    def din(name, shape, dt=F32):
        return nc.dram_tensor(name, list(shape), dt, kind="ExternalInput").ap()

    import os
    keep = os.environ.get("DBG_OUT", "").split(",")

    def dscr(name, shape, dt):
        ext = debug and (keep == [""] or name in keep)
        return nc.dram_tensor(name, list(shape), dt, kind=("ExternalOutput" if ext else "Internal")).ap()

    x = din("x", [T, D])
    gam = din("gam", [5, D])
    cst = din("cst", [128, NCST])
    sel = din("sel", [16, 2048])
    dn_win = din("dn_win", [128, 32, 8, 128])
    dn_wg = din("dn_wg", [128, 8, 16])
    dn_cw = din("dn_cw", [128, 24, 4])
    dn_vec = din("dn_vec", [16])
    dn_ong = din("dn_ong", [128, 1])
    dn_wout = din("dn_wout", [128, 8, 1024])
    mb_wqk = din("mb_wqk", [128, 16, 8, 128])
    mb_wv = din("mb_wv", [128, 8, 1024])
    mb_wout = din("mb_wout", [128, 8, 1024])
    wup = din("wup", [2, 128, 32, 8, 128])
    wdn = din("wdn", [2, 128, 32, 1024])
    out = nc.dram_tensor("out", [T, D], F32, kind="ExternalOutput").ap()

    qT_s = dscr("qT_s", [128, 8, T], F32)
    kT_s = dscr("kT_s", [128, 8, T], F32)
    vT_s = dscr("vT_s", [128, 8, T], F32)
    szT_s = dscr("szT_s", [128, 8, T], BF16)
    graw_s = dscr("graw_s", [128, NT, 16], F32)
    ss_s = dscr("ss_s", [128, NT, 16], F32)
    oT_s = dscr("oT_s", [128, 8, T], BF16)
    h1_s = dscr("h1_s", [T, D], F32)
    h2_s = dscr("h2_s", [T, D], F32)
    h3_s = dscr("h3_s", [T, D], F32)
    mq_s = dscr("mq_s", [128, 8, T], BF16)
    mk_s = dscr("mk_s", [128, 8, T], BF16)
    mv_s = dscr("mv_s", [T, 8, 129], BF16)
    msel_s = dscr("msel_s", [8, 16, T], BF16)
    o2T_s = dscr("o2T_s", [128, 8, T], BF16)

    only = os.environ.get('ONLY')
    run_ = lambda n: upto >= n and (only is None or int(only) == n)
    if run_(1) and not os.environ.get('SKIP1'):
        phase_gdn_proj(nc, x, gam, cst, dn_win, dn_wg, dn_cw, qT_s, kT_s, vT_s, szT_s, graw_s, ss_s)
    if run_(2):
        phase_gdn_core(nc, x, cst, sel, dn_vec, dn_ong, dn_wout, qT_s, kT_s, vT_s, szT_s, graw_s, ss_s, h1_s)
    if run_(3):
        phase_mlp(nc, "m0", h1_s, h2_s, gam[1], None, cst, wup[0], wdn[0])
    if run_(4):
        phase_moba_proj(nc, h2_s, gam, cst, mb_wqk, mb_wv, mq_s, mk_s, mv_s, msel_s)
    if run_(5):
        phase_moba_attn(nc, cst, sel, mq_s, mk_s, mv_s, msel_s, o2T_s)
    if run_(6):
        phase_outproj(nc, o2T_s, mb_wout, h2_s, h3_s)
    if run_(7):
        phase_mlp(nc, "m1", h3_s, out, gam[3], gam[4], cst, wup[1], wdn[1])
    return nc


def phase_gdn_proj(nc, x, gam, cst, dn_win, dn_wg, dn_cw, qT_s, kT_s, vT_s, szT_s, graw_s, ss_s):
    with ExitStack() as es:
        sb = lambda n, s, d: _sb(nc, es, n, s, d)
        W = sb("W", [128, 32, 8, 128], BF16)
        Wg = sb("Wg", [128, 8, 16], BF16)
        gamt = sb("gamt", [128, D], F32)
        cw = sb("cw", [128, 24, 4], F32)
        cs = sb("cs", [128, 640], F32)
        identb = sb("identb", [128, 128], BF16)
        halo = sb("halo", [128, 24, 3], F32)
        graw = sb("graw", [128, NT, 16], F32)
        ssraw = sb("ssraw", [128, NT, 16], F32)
        xin = Rot([(sb(f"xin{i}", [128, D], F32), f"xin{i}") for i in range(2)])
        hn = Rot([(sb(f"hn{i}", [128, D], BF16), f"hn{i}") for i in range(2)])
        stat = Rot([(sb(f"stat{i}", [128, 4], F32), f"stat{i}") for i in range(2)])
        junk = sb("junk", [128, D], BF16)
        hnT = Rot([(sb(f"hnT{i}", [128, 8, 512], BF16), f"hnT{i}") for i in range(2)])
        praw = Rot([(sb(f"praw{i}", [128, 515], F32), f"praw{i}") for i in range(3)])
        yv = Rot([(sb(f"yv{i}", [128, 512], F32), f"yv{i}") for i in range(3)])
        sy = Rot([(sb(f"sy{i}", [128, 512], F32), f"sy{i}") for i in range(3)])
        sq = Rot([(sb(f"sq{i}", [128, 512], F32), f"sq{i}") for i in range(2)])
        szb = Rot([(sb(f"szb{i}", [128, 512], BF16), f"szb{i}") for i in range(2)])
        ptr = Rot([(_ps(nc, es, f"ptr{i}", [128, 1024], BF16), f"PS:ptr{i}") for i in range(2)])
        pmm = Rot([(_ps(nc, es, f"pmm{i}", [128, 512], F32), f"PS:pmm{i}") for i in range(4)])
        pss = _ps(nc, es, "pss", [128, 512], F32)[:, 0:64].rearrange("p (j c) -> p j c", j=4)
        pg = _ps(nc, es, "pg", [128, 512], F32)[:, 0:64].rearrange("p (j c) -> p j c", j=4)

        P = Phase(nc, "gp")
        P.dma('sp', cs[:], cst[:, 0:640], w=['cs'], key='cs')
        P.dma('sp', gamt[:], gam[0].partition_broadcast(128), w=['gam'], key='gam')
        P.dma('sp', cw[:], dn_cw, w=['cw'], key='cw')
        for i in range(8):
            P.dma('pool', W[:, 4 * i:4 * i + 4], dn_win[:, 4 * i:4 * i + 4], w=[f'W{i}'], key=f'W{i}')
        P.dma('pool', Wg[:], dn_wg, w=['Wg'], key='Wg')
        P.copy('dve', identb[:], cs[:, C_ID:C_ID + 128], r=['cs'], w=['identb'])
        P.memset('pool', halo[:], 0.0, w=['halo%d' % c_ for c_ in range(24)])
        ones_col = cs[:, C_ONE:C_ONE + 1]
        for tb in range(8):
            hT, hTr = hnT.next()
            for j in range(4):
                t = tb * 4 + j
                emit_norm_T(P, x[t * 128:(t + 1) * 128, :], gamt, xin.next(), hn.next(), stat.next(), junk,
                            ptr.next(), hT[:, :, j * 128:(j + 1) * 128], identb, "gp", 'act' if j % 2 else 'dve',
                            f"{hTr}_{j}")
            hres = [f"{hTr}_{j}" for j in range(4)]
            for j in range(4):
                for c in range(8):
                    P.mm(pg[:, j, :], hT[:, c, j * 128:(j + 1) * 128], Wg[:, c, :], start=(c == 0), stop=(c == 7),
                         r=[hres[j], 'Wg'], w=['PS:pg'])
            P.copy('act', graw[:, tb * 4:tb * 4 + 4, :], pg, r=['PS:pg'], w=['graw'])
            for cc in range(32):
                pm, pmr = pmm.next()
                for c in range(8):
                    P.mm(pm[:], W[:, cc, c, :], hT[:, c, :], start=(c == 0), stop=(c == 7),
                         r=hres + [f'W{cc // 4}'], w=[pmr])
                h = cc % 8
                sl = slice(tb * 512, (tb + 1) * 512)
                if cc < 24:
                    pr_, prr = praw.next()
                    y, yr = yv.next()
                    s, sr = sy.next()
                    ce = 'dve'
                    P.copy('act', pr_[:, 3:515], pm[:], r=[pmr], w=[prr])
                    P.copy(ce, pr_[:, 0:3], halo[:, cc, :], r=['halo%d' % cc], w=[prr])
                    P.copy(ce, halo[:, cc, :], pr_[:, 512:515], r=[prr], w=['halo%d' % cc])
                    P.ts(ce, y[:], pr_[:, 0:512], cw[:, cc, 0:1], None, ALU.mult, r=[prr, 'cw'], w=[yr])
                    for k in range(1, 4):
                        P.stt(ce, y[:], pr_[:, k:k + 512], cw[:, cc, k:k + 1], y[:], ALU.mult, ALU.add,
                              r=[prr, 'cw', yr], w=[yr])
                    P.act(s[:], y[:], AF.Silu, r=[yr], w=[sr])
                    dst = (qT_s, kT_s, vT_s)[cc // 8]
                    P.dma('sp', dst[:, h, sl], s[:], r=[sr], w=[], key=sr)
                    if cc < 16:
                        q2, q2r = sq.next()
                        P.tt('pool', q2[:], s[:], s[:], ALU.mult, r=[sr], w=[q2r])
                        for j in range(4):
                            P.mm(pss[:, j, cc:cc + 1], q2[:, j * 128:(j + 1) * 128], ones_col, r=[q2r, 'cs'], w=['PS:pss'])
                        if cc == 15:
                            P.copy('dve', ssraw[:, tb * 4:tb * 4 + 4, :], pss, r=['PS:pss'], w=['ssraw'])
                else:
                    zb, zr = szb.next()
                    P.act(zb[:], pm[:], AF.Silu, r=[pmr], w=[zr])
                    P.dma('sp', szT_s[:, h, sl], zb[:], r=[zr], w=[], key=zr)
        P.dma('sp', graw_s, graw[:], r=['graw'], key='graw_o')
        P.dma('sp', ss_s, ssraw[:], r=['ssraw'], key='ss_o')
        P.run()


def phase_gdn_core(nc, x, cst, sel, dn_vec, dn_ong, dn_wout, qT_s, kT_s, vT_s, szT_s, graw_s, ss_s, h1_s):
    with ExitStack() as es:
        sb = lambda n, s, d: _sb(nc, es, n, s, d)
        cs = sb("cs", [128, 640], F32)
        identb = sb("identb", [128, 128], BF16)
        wout = sb("wout", [128, 8, 1024], BF16)
        ong = sb("ong", [128, 1], F32)
        vecb = sb("vecb", [128, 16], F32)
        graw = sb("graw", [128, NT, 16], F32)
        ssr = sb("ssr", [128, NT, 16], F32)
        G = {n: sb("g_" + n, [128, NT, 8], F32) for n in
             ("l1", "g", "gc", "gl", "lrq", "lrk", "bj", "ckbg", "ckdec", "cvb", "co1", "egt", "tmp", "tmp2")}
        rsrc = sb("rsrc", [128, NT, 16], F32)
        S = sb("S", [128, 8, 128], F32)
        qt_ = Rot([(sb(f"q{i}", [128, 8, 128], F32), f"q{i}") for i in range(2)])
        kt_ = Rot([(sb(f"k{i}", [128, 8, 128], F32), f"k{i}") for i in range(2)])
        vt_ = Rot([(sb(f"v{i}", [128, 8, 128], F32), f"v{i}") for i in range(2)])
        zt_ = Rot([(sb(f"z{i}", [128, 8, 128], BF16), f"z{i}") for i in range(2)])
        xt_ = Rot([(sb(f"x{i}", [128, D], F32), f"x{i}") for i in range(2)])
        ho_ = Rot([(sb(f"ho{i}", [128, D], F32), f"ho{i}") for i in range(2)])
        oTg = Rot([(sb(f"oTg{i}", [128, 8, 128], BF16), f"oTg{i}") for i in range(2)])
        names = ("kbg", "kdec", "vb", "DmU", "DmB", "M", "attnT", "A", "PA0", "PA1", "PM0", "PM1", "N0", "N1",
                 "nwT", "vnew", "O2", "o", "on", "dg1", "dg2", "usb", "ktm")
        Wk = {n: [sb(f"w_{n}{h}", [128, 128], F32) for h in range(8)] for n in names}
        ojunk = sb("ojunk", [128, 128], F32)
        ost = sb("ost", [128, 8, 4], F32)
        banks = [_ps(nc, es, f"bk{i}", [128, 512], F32) for i in range(8)]
        pbanks = Rot([(banks[i], f"PS:bk{i}") for i in range(6)])
        pbig = Rot([(banks[6 + i], f"PS:pb{i}") for i in range(2)])

        P = Phase(nc, "gc")
        P.dma('sp', cs[:], cst[:, 0:640], w=['cs'], key='cs')
        P.dma('sp', ong[:], dn_ong, w=['ong'], key='ong')
        P.dma('sp', vecb[:], dn_vec.partition_broadcast(128), w=['vecb'], key='vecb')
        P.dma('sp', graw[:], graw_s, w=['graw'], key='graw')
        P.dma('sp', ssr[:], ss_s, w=['ssr'], key='ssr')
        P.dma('pool', wout[:], dn_wout, w=['wout'], key='wout')
        P.copy('dve', identb[:], cs[:, C_ID:C_ID + 128], r=['cs'], w=['identb'])
        P.memset('pool', S[:], 0.0, w=[f'S{h}' for h in range(8)])
        identf = cs[:, C_ID:C_ID + 128]
        maskU = cs[:, C_MU:C_MU + 128]
        maskSU = cs[:, C_MSU:C_MSU + 128]
        tri = cs[:, C_TRI:C_TRI + 128]
        ones = cs[:, C_ONE:C_ONE + 128]

        b_raw = graw[:, :, 0:8]
        a_raw = graw[:, :, 8:16]
        gr = ['graw', 'vecb', 'ssr', 'cs']
        P.act(G["tmp"][:], b_raw, AF.Exp, scale=-1.0, r=gr, w=['g_tmp'])
        P.act(G["l1"][:], G["tmp"][:], AF.Ln, bias=1.0, r=['g_tmp'], w=['g_l1'])
        P.act(G["cvb"][:], G["l1"][:], AF.Exp, scale=-1.0, r=['g_l1'], w=['g_cvb'])
        for t in range(NT):
            P.tt('dve', G["tmp2"][:, t, :], a_raw[:, t, :], vecb[:, 8:16], ALU.add, r=gr, w=['g_tmp2'])
        P.act(G["tmp"][:], G["tmp2"][:], AF.Exp, r=['g_tmp2'], w=['g_tmp'])
        P.act(G["tmp2"][:], G["tmp"][:], AF.Ln, bias=1.0, r=['g_tmp'], w=['g_tmp2'])
        P.act(vecb[:, 0:8], vecb[:, 0:8], AF.Exp, r=['vecb'], w=['vecb2'])
        for t in range(NT):
            P.stt('dve', G["g"][:, t, :], G["tmp2"][:, t, :], -1.0, vecb[:, 0:8], ALU.mult, ALU.mult,
                  r=['g_tmp2', 'vecb2'], w=['g_g'])
        g2 = G["g"][:].rearrange("p t h -> p (t h)")
        pb, pbr = pbig.next()
        P.mm(pb[:, 0:256], tri, g2, r=['g_g', 'cs'], w=[pbr])
        P.copy('dve', G["gc"][:].rearrange("p t h -> p (t h)"), pb[:, 0:256], r=[pbr], w=['g_gc'])
        pb2, pb2r = pbig.next()
        P.mm(pb2[:, 0:256], ones, g2, r=['g_g', 'cs'], w=[pb2r])
        P.copy('dve', G["gl"][:].rearrange("p t h -> p (t h)"), pb2[:, 0:256], r=[pb2r], w=['g_gl'])
        P.ts('dve', G["tmp"][:], ssr[:, :, 0:8], EPS, None, ALU.add, r=gr, w=['g_tmp'])
        P.act(G["tmp"][:], G["tmp"][:], AF.Ln, r=['g_tmp'], w=['g_tmp'])
        P.ts('dve', G["lrq"][:], G["tmp"][:], -0.5, float(np.log(128.0 ** -0.5)), ALU.mult, ALU.add, r=['g_tmp'], w=['g_lrq'])
        P.ts('dve', G["tmp"][:], ssr[:, :, 8:16], EPS, None, ALU.add, r=gr + ['g_lrq'], w=['g_tmp'])
        P.act(G["tmp"][:], G["tmp"][:], AF.Ln, r=['g_tmp'], w=['g_tmp'])
        P.ts('dve', G["lrk"][:], G["tmp"][:], -0.5, None, ALU.mult, r=['g_tmp'], w=['g_lrk'])
        P.tt('dve', rsrc[:, :, 0:8], G["gc"][:], G["lrq"][:], ALU.add, r=['g_gc', 'g_lrq'], w=['rsrc'])
        P.tt('dve', G["tmp"][:], G["gc"][:], G["l1"][:], ALU.subtract, r=['g_gc', 'g_l1', 'g_lrk'], w=['g_tmp'])
        P.tt('dve', rsrc[:, :, 8:16], G["tmp"][:], G["lrk"][:], ALU.add, r=['g_tmp', 'g_lrk'], w=['rsrc'])
        P.tt('dve', G["bj"][:], G["lrk"][:], G["gc"][:], ALU.subtract, r=['g_gc', 'g_lrk'], w=['g_bj'])
        P.act(G["ckbg"][:], rsrc[:, :, 8:16], AF.Exp, r=['rsrc'], w=['g_ckbg'])
        P.act(G["co1"][:], rsrc[:, :, 0:8], AF.Exp, r=['rsrc'], w=['g_co1'])
        P.tt('dve', G["tmp2"][:], G["gl"][:], G["bj"][:], ALU.add, r=['g_gl', 'g_bj', 'g_g'], w=['g_tmp2'])
        P.act(G["ckdec"][:], G["tmp2"][:], AF.Exp, r=['g_tmp2'], w=['g_ckdec'])
        P.act(G["egt"][:], G["gl"][:], AF.Exp, r=['g_gl'], w=['g_egt'])
        gall = ['g_ckbg', 'g_co1', 'g_ckdec', 'g_egt', 'g_cvb', 'g_bj', 'rsrc']

        def loads(t):
            sl = slice(t * 128, (t + 1) * 128)
            q, qr = qt_.next(); k, kr = kt_.next(); v, vr = vt_.next(); z, zr = zt_.next(); xx, xr = xt_.next()
            P.dma('sp', q[:], qT_s[:, :, sl], w=[qr], key=qr)
            P.dma('sp', k[:], kT_s[:, :, sl], w=[kr], key=kr)
            P.dma('sp', v[:], vT_s[:, :, sl], w=[vr], key=vr)
            P.dma('sp', z[:], szT_s[:, :, sl], w=[zr], key=zr)
            P.dma('sp', xx[:], x[sl, :], w=[xr], key=xr)
            return (q, qr, k, kr, v, vr, z, zr, xx, xr)

        nxt = loads(0)
        import os
        HS = list(range(int(os.environ.get('GC_HEADS', 8))))
        NTL = int(os.environ.get('GC_TILES', NT))
        STG = int(os.environ.get('GC_STAGE', 99))
        groups = [HS[i:i + 4] for i in range(0, len(HS), 4)]
        rs = lambda n, h: f"w_{n}{h}"

        def stage(mmfn, consfns):
            for grp in groups:
                bk, bkr = pbanks.next()
                for i, h in enumerate(grp):
                    mmfn(h, bk[:, i * 128:(i + 1) * 128], bkr)
                for cf in consfns:
                    for i, h in enumerate(grp):
                        cf(h, bk[:, i * 128:(i + 1) * 128], bkr)

        for t in range(NTL):
            q, qr, k, kr, v, vr, z, zr, xx, xr = nxt
            if t + 1 < NTL:
                nxt = loads(t + 1)
            if STG < 1:
                continue
            stage(lambda h, pq, br: P.tr(pq, k[:, h, :], identf, r=[kr, 'cs'], w=[br]),
                  [lambda h, pq, br: P.copy('act', Wk["ktm"][h][:], pq, r=[br], w=[rs("ktm", h)])])
            for h in HS:
                P.ts('pool', Wk["kbg"][h][:], Wk["ktm"][h][:], G["ckbg"][:, t, h:h + 1], None, ALU.mult,
                     r=[rs("ktm", h)] + gall, w=[rs("kbg", h)])
                P.ts('pool', Wk["kdec"][h][:], Wk["ktm"][h][:], G["ckdec"][:, t, h:h + 1], None, ALU.mult,
                     r=[rs("ktm", h)] + gall, w=[rs("kdec", h)])
            stage(lambda h, pq, br: P.tr(pq, v[:, h, :], identf, r=[vr, 'cs'], w=[br]),
                  [lambda h, pq, br: P.act(Wk["vb"][h][:], pq, AF.Copy, scale=G["cvb"][:, t, h:h + 1], r=[br] + gall,
                                           w=[rs("vb", h)])])
            if STG < 2:
                continue
            for dgn, col0, msk, dst in (("dg1", 0, maskU, "DmU"), ("dg2", 8, maskSU, "DmB")):
                for h in HS:
                    P.ts('pool', Wk[dgn][h][:], identf, rsrc[:, t, col0 + h:col0 + h + 1], None, ALU.mult,
                         r=['cs', 'rsrc'], w=[rs(dgn, h)])
                stage(lambda h, pq, br, dgn=dgn: P.mm(pq, ones, Wk[dgn][h][:], r=['cs', rs(dgn, h)], w=[br]),
                      [lambda h, pq, br, dst=dst, msk=msk: P.stt('dve', Wk[dst][h][:], pq, G["bj"][:, t, h:h + 1], msk,
                                                               ALU.add, ALU.add, r=[br, 'cs'] + gall, w=[rs(dst, h)])])
                for h in HS:
                    P.act(Wk[dst][h][:], Wk[dst][h][:], AF.Exp, r=[rs(dst, h)], w=[rs(dst, h)])
            stage(lambda h, pq, br: P.mm(pq, k[:, h, :], k[:, h, :], r=[kr], w=[br]),
                  [lambda h, pq, br: P.tt('dve', Wk["M"][h][:], pq, Wk["DmB"][h][:], ALU.mult, r=[br, rs("DmB", h)],
                                          w=[rs("M", h)])])
            stage(lambda h, pq, br: P.mm(pq, k[:, h, :], q[:, h, :], r=[kr, qr], w=[br]),
                  [lambda h, pq, br: P.tt('dve', Wk["attnT"][h][:], pq, Wk["DmU"][h][:], ALU.mult, r=[br, rs("DmU", h)],
                                          w=[rs("attnT", h)])])
            if STG < 3:
                continue
            stage(lambda h, pq, br: P.tr(pq, Wk["M"][h][:], identf, r=[rs("M", h), 'cs'], w=[br]),
                  [lambda h, pq, br: P.copy('act', Wk["A"][h][:], pq, r=[br], w=[rs("A", h)])])
            for h in HS:
                P.tt('pool', Wk["N0"][h][:], identf, Wk["M"][h][:], ALU.subtract, r=['cs', rs("M", h)], w=[rs("N0", h)])
            if STG < 4:
                continue
            PAc = {h: (Wk["A"][h], rs("A", h)) for h in HS}
            PMc = {h: (Wk["M"][h], rs("M", h)) for h in HS}
            Nc = {h: (Wk["N0"][h], rs("N0", h)) for h in HS}
            for lvl in range(6):
                last = (lvl == 5)
                npa = {h: (Wk["PA%d" % (lvl % 2)][h], rs("PA%d" % (lvl % 2), h)) for h in HS}
                npm = {h: (Wk["PM%d" % (lvl % 2)][h], rs("PM%d" % (lvl % 2), h)) for h in HS}
                nnn = {h: (Wk["N%d" % ((lvl + 1) % 2)][h], rs("N%d" % ((lvl + 1) % 2), h)) for h in HS}
                stage(lambda h, pq, br: P.mm(pq, PMc[h][0][:], PAc[h][0][:], r=[PMc[h][1], PAc[h][1]], w=[br]),
                      [lambda h, pq, br: P.copy('act', npa[h][0][:], pq, r=[br], w=[npa[h][1]])])
                if not last:
                    stage(lambda h, pq, br: P.mm(pq, PAc[h][0][:], PMc[h][0][:], r=[PMc[h][1], PAc[h][1]], w=[br]),
                          [lambda h, pq, br: P.copy('dve', npm[h][0][:], pq, r=[br], w=[npm[h][1]])])
                stage(lambda h, pq, br: P.mm(pq, npa[h][0][:], Nc[h][0][:], r=[npa[h][1], Nc[h][1]], w=[br]),
                      [lambda h, pq, br: P.tt('dve', nnn[h][0][:], pq, Nc[h][0][:], ALU.add, r=[br, Nc[h][1]],
                                              w=[nnn[h][1]])])
                PAc = npa
                if not last:
                    PMc = npm
                Nc = nnn
            if STG < 5:
                continue
            stage(lambda h, pq, br: P.mm(pq, Wk["kbg"][h][:], Nc[h][0][:], r=[rs("kbg", h), Nc[h][1]], w=[br]),
                  [lambda h, pq, br: P.ts('dve', Wk["nwT"][h][:], pq, -1.0, None, ALU.mult, r=[br], w=[rs("nwT", h)])])
            if STG < 6:
                continue
            stage(lambda h, pq, br: P.mm(pq, Nc[h][0][:], Wk["vb"][h][:], r=[Nc[h][1], rs("vb", h)], w=[br]),
                  [lambda h, pq, br: P.copy('act', Wk["usb"][h][:], pq, r=[br], w=[rs("usb", h)])])
            stage(lambda h, pq, br: P.mm(pq, Wk["nwT"][h][:], S[:, h, :], r=[rs("nwT", h), f'S{h}'], w=[br]),
                  [lambda h, pq, br: P.tt('dve', Wk["vnew"][h][:], pq, Wk["usb"][h][:], ALU.add, r=[br, rs("usb", h)],
                                          w=[rs("vnew", h)])])
            stage(lambda h, pq, br: P.mm(pq, Wk["attnT"][h][:], Wk["vnew"][h][:], r=[rs("attnT", h), rs("vnew", h)], w=[br]),
                  [lambda h, pq, br: P.copy('act', Wk["O2"][h][:], pq, r=[br], w=[rs("O2", h)])])
            stage(lambda h, pq, br: P.mm(pq, q[:, h, :], S[:, h, :], r=[qr, f'S{h}'], w=[br]),
                  [lambda h, pq, br: P.stt('dve', Wk["o"][h][:], pq, G["co1"][:, t, h:h + 1], Wk["O2"][h][:], ALU.mult,
                                           ALU.add, r=[br, rs("O2", h)] + gall, w=[rs("o", h)])])
            stage(lambda h, pq, br: P.mm(pq, Wk["kdec"][h][:], Wk["vnew"][h][:], r=[rs("kdec", h), rs("vnew", h)], w=[br]),
                  [lambda h, pq, br: P.stt('dve', S[:, h, :], S[:, h, :], G["egt"][:, t, h:h + 1], pq, ALU.mult, ALU.add,
                                           r=[br, f'S{h}'] + gall, w=[f'S{h}'])])
            for h in HS:
                P.act(ojunk[:], Wk["o"][h][:], AF.Square, accum=ost[:, h, 0:1], r=[rs("o", h)], w=['ost'])
            if STG < 7:
                continue
            P.ts('dve', ost[:, :, 1:2], ost[:, :, 0:1], 1.0 / 128, EPS, ALU.mult, ALU.add, r=['ost'], w=['ost'])
            P.act(ost[:, :, 2:3], ost[:, :, 1:2], AF.Sqrt, r=['ost'], w=['ost'])
            P.op('dve', lambda e: e.reciprocal(out=ost[:, :, 3:4], in_=ost[:, :, 2:3]), ['ost'], ['ost'])
            og, ogr = oTg.next()
            for h in HS:
                P.ts('pool', Wk["on"][h][:], Wk["o"][h][:], ost[:, h, 3:4], None, ALU.mult, r=[rs("o", h), 'ost'], w=[rs("on", h)])
            stage(lambda h, pq, br: P.tr(pq, Wk["on"][h][:], identf, r=[rs("on", h), 'cs'], w=[br]),
                  [lambda h, pq, br: P.stt('dve', og[:, h, :], pq, ong[:, 0:1], z[:, h, :], ALU.mult, ALU.mult,
                                           r=[br, 'ong', zr], w=[ogr])])
            if STG < 8:
                continue
            ho, hor = ho_.next()
            for half in range(2):
                pb, pbr = pbig.next()
                for h in HS:
                    P.mm(pb[:], og[:, h, :], wout[:, h, half * 512:(half + 1) * 512], start=(h == 0), stop=(h == HS[-1]),
                         r=[ogr, 'wout'], w=[pbr])
                P.tt('dve', ho[:, half * 512:(half + 1) * 512], pb[:], xx[:, half * 512:(half + 1) * 512], ALU.add,
                     r=[pbr, xr], w=[hor])
            P.dma('sp', h1_s[t * 128:(t + 1) * 128, :], ho[:], r=[hor], key=hor)
        P.run()


def phase_mlp(nc, name, h_in, h_out, gam_mlp, gam_final, cst, wup_l, wdn_l):
    with ExitStack() as es:
        sb = lambda n, s, d: _sb(nc, es, n, s, d)
        Wu = sb("Wu", [128, 32, 8, 128], BF16)
        Wd = sb("Wd", [128, 32, 1024], BF16)
        gamt = sb("gamt", [128, D], F32)
        gamf = sb("gamf", [128, D], F32) if gam_final is not None else None
        idf = sb("idf", [128, 128], F32)
        identb = sb("identb", [128, 128], BF16)
        a1T = sb("a1T", [128, 32, 256], BF16)
        hmid = Rot([(sb(f"hmid{i}", [128, D], F32), f"hmid{i}") for i in range(4)])
        hn = Rot([(sb(f"hn{i}", [128, D], BF16), f"hn{i}") for i in range(2)])
        stat = Rot([(sb(f"stat{i}", [128, 4], F32), f"stat{i}") for i in range(2)])
        fstat = Rot([(sb(f"fstat{i}", [128, 4], F32), f"fstat{i}") for i in range(2)])
        junk = sb("junk", [128, D], BF16)
        hnT = Rot([(sb(f"hnT{i}", [128, 8, 256], BF16), f"hnT{i}") for i in range(2)])
        rl = Rot([(sb(f"rl{i}", [128, 256], F32), f"rl{i}") for i in range(3)])
        ost = Rot([(sb(f"ost{i}", [128, D], F32), f"ost{i}") for i in range(2)])
        ptr = Rot([(_ps(nc, es, f"ptr{i}", [128, 1024], BF16), f"PS:ptr{i}") for i in range(2)])
        pup = Rot([(_ps(nc, es, f"pup{i}", [128, 512], F32), f"PS:pup{i}") for i in range(3)])
        pdn = Rot([(_ps(nc, es, f"pdn{i}", [128, 512], F32), f"PS:pdn{i}") for i in range(3)])

        P = Phase(nc, name)
        P.dma('sp', idf[:], cst[:, C_ID:C_ID + 128], w=['idf'], key='idf')
        P.dma('sp', gamt[:], gam_mlp.partition_broadcast(128), w=['gam'], key='gam')
        if gamf is not None:
            P.dma('sp', gamf[:], gam_final.partition_broadcast(128), w=['gamf'], key='gamf')
        for i in range(8):
            P.dma('pool', Wu[:, 4 * i:4 * i + 4], wup_l[:, 4 * i:4 * i + 4], w=[f'Wu{i}'], key=f'Wu{i}')
        for i in range(8):
            P.dma('pool', Wd[:, 4 * i:4 * i + 4], wdn_l[:, 4 * i:4 * i + 4], w=[f'Wd{i}'], key=f'Wd{i}')
        P.copy('dve', identb[:], idf[:], r=['idf'], w=['identb'])
        for tb in range(16):
            hT, hTr = hnT.next()
            hm = []
            for j in range(2):
                t = tb * 2 + j
                xm = hmid.next()
                hm.append(xm)
                emit_norm_T(P, h_in[t * 128:(t + 1) * 128, :], gamt, xm, hn.next(), stat.next(), junk,
                            ptr.next(), hT[:, :, j * 128:(j + 1) * 128], identb, name, 'act' if j % 2 else 'dve',
                            f"{hTr}_{j}")
            hres = [f"{hTr}_{j}" for j in range(2)]
            for f in range(32):
                pu, pur = pup.next()
                for c in range(8):
                    P.mm(pu[:, 0:256], Wu[:, f, c, :], hT[:, c, :], start=(c == 0), stop=(c == 7),
                         r=hres + [f'Wu{f // 4}'], w=[pur])
                r_, rr = rl.next()
                P.act(r_[:], pu[:, 0:256], AF.Relu, r=[pur], w=[rr])
                P.tt('pool', a1T[:, f, :], r_[:], r_[:], ALU.mult, r=[rr], w=[f'a1T{f}'])
            ares = [f'a1T{f}' for f in range(32)]
            for j in range(2):
                t = tb * 2 + j
                xm, xmr = hm[j]
                o_, orr = ost.next()
                for half in range(2):
                    pd, pdr = pdn.next()
                    for f in range(32):
                        P.mm(pd[:], a1T[:, f, j * 128:(j + 1) * 128], Wd[:, f, half * 512:(half + 1) * 512],
                             start=(f == 0), stop=(f == 31), r=[f'a1T{f}', f'Wd{f // 4}'], w=[pdr])
                    P.tt('dve', o_[:, half * 512:(half + 1) * 512], pd[:], xm[:, half * 512:(half + 1) * 512], ALU.add,
                         r=[pdr, xmr], w=[orr])
                if gamf is not None:
                    fs, fsr = fstat.next()
                    P.act(junk[:], o_[:], AF.Square, accum=fs[:, 0:1], r=[orr], w=[fsr])
                    P.ts('dve', fs[:, 1:2], fs[:, 0:1], 1.0 / D, EPS, ALU.mult, ALU.add, r=[fsr], w=[fsr])
                    P.act(fs[:, 2:3], fs[:, 1:2], AF.Sqrt, r=[fsr], w=[fsr])
                    P.op('dve', (lambda fs=fs: (lambda e: e.reciprocal(out=fs[:, 3:4], in_=fs[:, 2:3])))(), [fsr], [fsr])
                    P.stt('dve', o_[:], o_[:], fs[:, 3:4], gamf[:], ALU.mult, ALU.mult, r=[orr, fsr, 'gamf'], w=[orr])
                P.dma('sp', h_out[t * 128:(t + 1) * 128, :], o_[:], r=[orr], key=orr)
        P.run()


def phase_moba_proj(nc, h_in, gam, cst, mb_wqk, mb_wv, mq_s, mk_s, mv_s, msel_s):
    with ExitStack() as es:
        sb = lambda n, s, d: _sb(nc, es, n, s, d)
        W = sb("W", [128, 16, 8, 128], BF16)
        Wv = sb("Wv", [128, 8, 1024], BF16)
        gamt = sb("gamt", [128, D], F32)
        cs = sb("cs", [128, NCST], F32)
        identb = sb("identb", [128, 128], BF16)
        kms = sb("kms", [128, 8, 16], F32)
        xin = Rot([(sb(f"xin{i}", [128, D], F32), f"xin{i}") for i in range(2)])
        hn = Rot([(sb(f"hn{i}", [128, D], BF16), f"hn{i}") for i in range(2)])
        stat = Rot([(sb(f"stat{i}", [128, 4], F32), f"stat{i}") for i in range(2)])
        junk = sb("junk", [128, D], BF16)
        hnT = Rot([(sb(f"hnT{i}", [128, 8, 512], BF16), f"hnT{i}") for i in range(2)])
        kblk = Rot([(sb(f"kblk{i}", [128, 8, 512], BF16), f"kblk{i}") for i in range(2)])
        qblk = Rot([(sb(f"qblk{i}", [128, 8, 512], BF16), f"qblk{i}") for i in range(2)])
        q32 = Rot([(sb(f"q32{i}", [128, 512], F32), f"q32{i}") for i in range(2)])
        selb = Rot([(sb(f"selb{i}", [128, 4, 8, 16], F32), f"selb{i}") for i in range(2)])
        selTb = Rot([(sb(f"selTb{i}", [128, 512], BF16), f"selTb{i}") for i in range(2)])
        gm = Rot([(sb(f"gm{i}", [128, 16], F32), f"gm{i}") for i in range(4)])
        t8 = Rot([(sb(f"t8{i}", [128, 8], F32), f"t8{i}") for i in range(4)])
        vaug = Rot([(sb(f"vaug{i}", [128, 8, 129], BF16), f"vaug{i}") for i in range(2)])
        ptr = Rot([(_ps(nc, es, f"ptr{i}", [128, 1024], BF16), f"PS:ptr{i}") for i in range(2)])
        pmm = Rot([(_ps(nc, es, f"pmm{i}", [128, 512], F32), f"PS:pmm{i}") for i in range(3)])
        pgt = _ps(nc, es, "pgt", [128, 512], F32)
        pst = _ps(nc, es, "pst", [128, 512], F32)

        P = Phase(nc, "mp")
        P.dma('sp', cs[:], cst, w=['cs'], key='cs')
        P.dma('sp', gamt[:], gam[2].partition_broadcast(128), w=['gam'], key='gam')
        for i in range(4):
            P.dma('pool', W[:, 4 * i:4 * i + 4], mb_wqk[:, 4 * i:4 * i + 4], w=[f'W{i}'], key=f'W{i}')
        P.dma('pool', Wv[:], mb_wv, w=['Wv'], key='Wv')
        P.copy('dve', identb[:], cs[:, C_ID:C_ID + 128], r=['cs'], w=['identb'])
        P.memset('pool', kms[:], 0.0, w=['kms'])
        for i in range(2):
            va, var = vaug.next()
            P.memset('pool', va[:], 1.0, w=[var])
        identf = cs[:, C_ID:C_ID + 128]
        scale = 128.0 ** -0.5
        for tb in range(8):
            hT, hTr = hnT.next()
            for j in range(4):
                t = tb * 4 + j
                emit_norm_T(P, h_in[t * 128:(t + 1) * 128, :], gamt, xin.next(), hn.next(), stat.next(), junk,
                            ptr.next(), hT[:, :, j * 128:(j + 1) * 128], identb, "mp", 'act' if j % 2 else 'dve',
                            f"{hTr}_{j}")
            hres = [f"{hTr}_{j}" for j in range(4)]
            sl = slice(tb * 512, (tb + 1) * 512)
            kb, kbr = kblk.next()
            for h in range(8):
                pm, pmr = pmm.next()
                for c in range(8):
                    P.mm(pm[:], W[:, 8 + h, c, :], hT[:, c, :], start=(c == 0), stop=(c == 7),
                         r=hres + [f'W{(8 + h) // 4}'], w=[pmr])
                P.copy('act', kb[:, h, :], pm[:], r=[pmr], w=[kbr])
                P.op('dve', (lambda pm=pm, h=h, tb=tb: (lambda e: e.tensor_reduce(
                    out=kms[:, h, 2 * tb:2 * tb + 2], in_=pm[:].rearrange("p (a b) -> p a b", b=256),
                    axis=AX.X, op=ALU.add)))(), [pmr], ['kms'])
            P.dma('sp', mk_s[:, :, sl], kb[:], r=[kbr], key=kbr)
            qb_, qbr = qblk.next()
            sbt, sbr = selb.next()
            for h in range(8):
                pm, pmr = pmm.next()
                for c in range(8):
                    P.mm(pm[:], W[:, h, c, :], hT[:, c, :], start=(c == 0), stop=(c == 7),
                         r=hres + [f'W{h // 4}'], w=[pmr])
                qf, qfr = q32.next()
                P.copy('act', qf[:], pm[:], r=[pmr], w=[qfr])
                P.ts('dve', qb_[:, h, :], pm[:], scale, None, ALU.mult, r=[pmr], w=[qbr])
                for j in range(4):
                    t = tb * 4 + j
                    qblock = t // 2
                    P.mm(pgt[:, (h * 4 + j) * 16:(h * 4 + j) * 16 + 16], qf[:, j * 128:(j + 1) * 128], kms[:, h, :],
                         r=[qfr, 'kms'], w=['PS:pgt'])
                for j in range(4):
                    t = tb * 4 + j
                    qblock = t // 2
                    g_, gr_ = gm.next()
                    t8_, t8r = t8.next()
                    P.tt('dve', g_[:], pgt[:, (h * 4 + j) * 16:(h * 4 + j) * 16 + 16],
                         cs[:, C_PAST + qblock * 16:C_PAST + qblock * 16 + 16], ALU.add, r=['PS:pgt', 'cs'], w=[gr_])
                    P.op('dve', (lambda t8_=t8_, g_=g_: (lambda e: e.max(out=t8_[:], in_=g_[:])))(), [gr_], [t8r])
                    P.ts('dve', g_[:], g_[:], t8_[:, 2:3], None, ALU.is_ge, r=[gr_, t8r], w=[gr_])
                    P.ts('dve', sbt[:, j, h, :], g_[:], 1.0, -NEG, ALU.subtract, ALU.mult, r=[gr_], w=[sbr])
            P.dma('sp', mq_s[:, :, sl], qb_[:], r=[qbr], key=qbr)
            stb, stbr = selTb.next()
            for j in range(4):
                P.tr(pst[:, j * 128:(j + 1) * 128], sbt[:, j].rearrange("p h n -> p (h n)"), identf, r=[sbr, 'cs'], w=['PS:pst'])
            P.copy('act', stb[:], pst[:], r=['PS:pst'], w=[stbr])
            P.dma('sp', msel_s[:, :, sl].rearrange("h n t -> (h n) t"), stb[:], r=[stbr], key=stbr)
            for j in range(4):
                t = tb * 4 + j
                va, var = vaug.next()
                for half in range(2):
                    pm, pmr = pmm.next()
                    for c in range(8):
                        P.mm(pm[:], hT[:, c, j * 128:(j + 1) * 128], Wv[:, c, half * 512:(half + 1) * 512],
                             start=(c == 0), stop=(c == 7), r=[hres[j], 'Wv'], w=[pmr])
                    P.copy('act' if half else 'dve', va[:, half * 4:half * 4 + 4, 0:128],
                           pm[:].rearrange("p (h d) -> p h d", h=4), r=[pmr], w=[var])
                P.dma('sp', mv_s[t * 128:(t + 1) * 128], va[:], r=[var], key=var)
        P.run()


def phase_moba_attn(nc, cst, sel, mq_s, mk_s, mv_s, msel_s, o2T_s):
    with ExitStack() as es:
        sb = lambda n, s, d: _sb(nc, es, n, s, d)
        cs = sb("cs", [128, NCST], F32)
        selt = sb("selt", [16, 2048], F32)
        selb16 = sb("selb16", [16, 2048], BF16)
        identb = sb("identb", [128, 128], BF16)
        causb = sb("causb", [128, 128], BF16)
        qh = Rot([(sb(f"qh{i}", [128, T], BF16), f"qh{i}") for i in range(2)])
        kh = Rot([(sb(f"kh{i}", [128, T], BF16), f"kh{i}") for i in range(2)])
        vh = Rot([(sb(f"vh{i}", [128, NT, 129], BF16), f"vh{i}") for i in range(2)])
        sh = Rot([(sb(f"sh{i}", [16, T], BF16), f"sh{i}") for i in range(2)])
        oTh = Rot([(sb(f"oTh{i}", [128, T], BF16), f"oTh{i}") for i in range(2)])
        pT = Rot([(sb(f"pT{i}", [128, 128], BF16), f"pT{i}") for i in range(6)])
        rden = Rot([(sb(f"rden{i}", [128, 1], F32), f"rden{i}") for i in range(2)])
        onb = Rot([(sb(f"onb{i}", [128, 128], BF16), f"onb{i}") for i in range(2)])
        banks = [_ps(nc, es, f"bk{i}", [128, 512], F32) for i in range(6)]
        sbanks = Rot([(banks[i], f"PS:sbk{i}") for i in range(4)])
        pacc = Rot([(banks[4 + i], f"PS:pacc{i}") for i in range(2)])
        ptr = Rot([(_ps(nc, es, f"ptr{i}", [128, 1024], BF16), f"PS:ptr{i}") for i in range(2)])

        P = Phase(nc, "ma")
        P.dma('sp', cs[:], cst, w=['cs'], key='cs')
        P.dma('sp', selt[:], sel, w=['selt'], key='selt')
        P.copy('dve', identb[:], cs[:, C_ID:C_ID + 128], r=['cs'], w=['identb'])
        P.copy('dve', causb[:], cs[:, C_MU:C_MU + 128], r=['cs'], w=['causb'])
        P.copy('dve', selb16[:], selt[:], r=['selt'], w=['selb16'])

        def loads(h):
            q, qr = qh.next(); k, kr = kh.next(); v, vr = vh.next(); s, sr = sh.next()
            P.dma('sp', q[:], mq_s[:, h, :], w=[qr], key=qr)
            P.dma('sp', k[:], mk_s[:, h, :], w=[kr], key=kr)
            for i in range(4):
                P.dma('sp', v[:, i * 8:(i + 1) * 8, :],
                      mv_s[i * 1024:(i + 1) * 1024, h, :].rearrange("(t p) d -> p t d", p=128),
                      w=[vr + f"_{i}"], key=vr + f"_{i}")
            P.dma('sp', s[:], msel_s[h], w=[sr], key=sr)
            return (q, qr, k, kr, v, vr, s, sr)

        nxt = loads(0)
        import os
        NHL = int(os.environ.get('MA_HEADS', 8))
        for h in range(NHL):
            q, qr, k, kr, v, vr, s, sr = nxt
            if h + 1 < NHL:
                nxt = loads(h + 1)
            oT, oTr = oTh.next()
            for qt in range(NT):
                qblock = qt // 2
                qs = slice(qt * 128, (qt + 1) * 128)
                pa, par = pacc.next()
                for g0 in range(0, qt + 1, 4):
                    kts = list(range(g0, min(g0 + 4, qt + 1)))
                    sbk, sbr_ = sbanks.next()
                    for i, kt in enumerate(kts):
                        b = kt // 2
                        ks = slice(kt * 128, (kt + 1) * 128)
                        p1 = sbk[:, i * 128:(i + 1) * 128]
                        nomask = (b == qblock and kt != qt)
                        P.mm(p1, k[:, ks], q[:, qs], start=True, stop=nomask, r=[kr, qr], w=[sbr_])
                        if b < qblock:
                            P.mm(p1, selb16[:, b * 128:(b + 1) * 128], s[:, qs], start=False, stop=True,
                                 r=['selb16', sr], w=[sbr_])
                        elif kt == qt:
                            P.mm(p1, identb[:], causb[:], start=False, stop=True, r=['identb', 'causb'], w=[sbr_])
                    for i, kt in enumerate(kts):
                        p1 = sbk[:, i * 128:(i + 1) * 128]
                        pt, ptr_ = pT.next()
                        P.act(pt[:], p1, AF.Exp, bias=cs[:, C_ALI + h * 32 + (qt - kt):C_ALI + h * 32 + (qt - kt) + 1],
                              r=[sbr_, 'cs'], w=[ptr_])
                        P.mm(pa[:, 0:129], pt[:], v[:, kt, :], start=(kt == 0), stop=(kt == qt),
                             r=[ptr_, vr + f"_{kt // 8}"], w=[par])
                rd, rdr = rden.next()
                ob, obr = onb.next()
                P.op('dve', (lambda rd=rd, pa=pa: (lambda e: e.reciprocal(out=rd[:], in_=pa[:, 128:129])))(), [par], [rdr])
                P.ts('dve', ob[:], pa[:, 0:128], rd[:, 0:1], None, ALU.mult, r=[par, rdr], w=[obr])
                pt2, pt2r = ptr.next()
                P.tr(pt2[:, 0:128], ob[:], identb[:], r=[obr, 'identb'], w=[pt2r])
                P.copy('dve', oT[:, qs], pt2[:, 0:128], r=[pt2r], w=[oTr])
            P.dma('sp', o2T_s[:, h, :], oT[:], r=[oTr], key=oTr)
        P.run()


def phase_outproj(nc, oT_s, wout_d, h_in, h_out):
    with ExitStack() as es:
        sb = lambda n, s, d: _sb(nc, es, n, s, d)
        wout = sb("wout", [128, 8, 1024], BF16)
        ot = Rot([(sb(f"ot{i}", [128, 8, 128], BF16), f"ot{i}") for i in range(3)])
        xt = Rot([(sb(f"xt{i}", [128, D], F32), f"xt{i}") for i in range(3)])
        ho = Rot([(sb(f"ho{i}", [128, D], F32), f"ho{i}") for i in range(2)])
        pb = Rot([(_ps(nc, es, f"pb{i}", [128, 512], F32), f"PS:pb{i}") for i in range(4)])
        P = Phase(nc, "op")
        P.dma('pool', wout[:], wout_d, w=['wout'], key='wout')
        for t in range(NT):
            sl = slice(t * 128, (t + 1) * 128)
            o, orr = ot.next(); xx, xr = xt.next(); hh, hr = ho.next()
            P.dma('sp', o[:], oT_s[:, :, sl], w=[orr], key=orr)
            P.dma('sp', xx[:], h_in[sl, :], w=[xr], key=xr)
            for half in range(2):
                p, pr = pb.next()
                for h in range(8):
                    P.mm(p[:], o[:, h, :], wout[:, h, half * 512:(half + 1) * 512], start=(h == 0), stop=(h == 7),
                         r=[orr, 'wout'], w=[pr])
                P.tt('dve', hh[:, half * 512:(half + 1) * 512], p[:], xx[:, half * 512:(half + 1) * 512], ALU.add,
                     r=[pr, xr], w=[hr])
            P.dma('sp', h_out[sl, :], hh[:], r=[hr], key=hr)
        P.run()


def make_consts():
    c = np.zeros((128, NCST), np.float32)
    j = np.arange(128)[:, None]
    i = np.arange(128)[None, :]
    c[:, C_ID:C_ID + 128] = (i == j)
    c[:, C_MU:C_MU + 128] = np.where(i >= j, 0.0, NEG)
    c[:, C_MSU:C_MSU + 128] = np.where(i > j, 0.0, NEG)
    c[:, C_TRI:C_TRI + 128] = (j <= i)
    c[:, C_ONE:C_ONE + 128] = 1.0
    p = np.arange(128, dtype=np.float64)
    for h in range(8):
        slope = 2.0 ** (-8.0 * (h + 1) / 8)
        for dlt in range(32):
            c[:, C_ALI + h * 32 + dlt] = slope * (p - dlt * 128.0)
    for qb in range(16):
        for n in range(16):
            c[:, C_PAST + qb * 16 + n] = 0.0 if n < qb else -1e30
    s = np.zeros((16, 2048), np.float32)
    for b in range(16):
        s[b, b * 128:(b + 1) * 128] = 1.0
    return c, s


def prep_shared(inp):
    f = lambda a: np.ascontiguousarray(a, dtype=np.float32)
    cst, sel = make_consts()
    w = inp["dn_w_in"][0]
    mw = inp["mb_w_in"][0]
    sh = {
        "gam": f(np.concatenate([inp["mix_norm_g"][0:1], inp["mlp_norm_g"][0:1], inp["mix_norm_g"][1:2],
                                 inp["mlp_norm_g"][1:2], inp["final_norm_g"][None, :]], axis=0)),
        "cst": cst, "sel": sel,
        "dn_win": f(w[:, :4096].reshape(8, 128, 32, 128).transpose(1, 2, 0, 3)),
        "dn_wg": f(w[:, 4096:].reshape(8, 128, 16).transpose(1, 0, 2)),
        "dn_cw": f(inp["dn_conv_w"][0].reshape(4, 24, 128).transpose(2, 1, 0)),
        "dn_vec": f(np.concatenate([inp["dn_a_log"][0], inp["dn_dt_bias"][0]])),
        "dn_ong": f(inp["dn_out_norm_g"][0].reshape(128, 1)),
        "dn_wout": f(inp["dn_w_out"][0].reshape(8, 128, 1024).transpose(1, 0, 2)),
        "mb_wqk": f(mw[:, :2048].reshape(8, 128, 16, 128).transpose(1, 2, 0, 3)),
        "mb_wv": f(mw[:, 2048:].reshape(8, 128, 1024).transpose(1, 0, 2)),
        "mb_wout": f(inp["mb_w_out"][0].reshape(8, 128, 1024).transpose(1, 0, 2)),
        "wup": f(inp["mlp_w_up"].reshape(2, 8, 128, 32, 128).transpose(0, 2, 3, 1, 4)),
        "wdn": f(inp["mlp_w_down"].reshape(2, 32, 128, 1024).transpose(0, 2, 1, 3)),
    }
    return sh


_NC_CACHE = {}


def kernel(**inputs):
    inp = {k: np.asarray(v) for k, v in inputs.items()}
    sh = prep_shared(inp)
    if "nc" not in _NC_CACHE:
        _NC_CACHE["nc"] = build()
    nc = _NC_CACHE["nc"]
    x = np.ascontiguousarray(inp["x"], dtype=np.float32)
    in_maps = [dict(sh, x=x[b]) for b in range(8)]
    res = run_bass_kernel_spmd(nc, in_maps, core_ids=list(range(8)))
    return np.stack([np.asarray(r["out"]) for r in res.results], axis=0).astype(np.float32)
```

```python
import numpy as np
from contextlib import ExitStack
import concourse.bass as bass
import concourse.mybir as mybir
from concourse.bass_utils import run_bass_kernel_spmd

F32 = mybir.dt.float32
BF16 = mybir.dt.bfloat16
AF = mybir.ActivationFunctionType
ALU = mybir.AluOpType
AX = mybir.AxisListType

T = 4096
D = 1024
NT = 32
H = 8
NEG = -30000.0
EPS = 1e-6

C_ID, C_MU, C_MSU, C_TRI, C_ONE, C_ALI, C_PAST = 0, 128, 256, 384, 512, 640, 896
NCST = 896 + 256


class Phase:
    def __init__(self, nc, name):
        self.nc = nc
        self.name = name
        self.ops = []

    def op(self, eng, fn, r=(), w=(), dma=False, key=None):
        self.ops.append(dict(eng=eng, fn=fn, reads=tuple(r), writes=tuple(w), dma=dma, key=key, signal=False))

    def mm(self, out, lhsT, rhs, start=True, stop=True, r=(), w=()):
        self.op('pe', lambda e: e.matmul(out, lhsT=lhsT, rhs=rhs, start=start, stop=stop), r, w)

    def tr(self, out, in_, ident, r=(), w=()):
        self.op('pe', lambda e: e.transpose(out=out, in_=in_, identity=ident), r, w)

    def act(self, out, in_, func, r=(), w=(), bias=None, scale=None, accum=None):
        kw = {}
        if bias is not None:
            kw['bias'] = bias
        if scale is not None:
            kw['scale'] = scale
        if accum is not None:
            kw['accum_out'] = accum
        self.op('act', lambda e: e.activation(out=out, in_=in_, func=func, **kw), r, w)

    def ts(self, eng, out, in0, s1, s2, op0, op1=None, r=(), w=()):
        if op1 is None:
            self.op(eng, lambda e: e.tensor_scalar(out=out, in0=in0, scalar1=s1, scalar2=None, op0=op0), r, w)
        else:
            self.op(eng, lambda e: e.tensor_scalar(out=out, in0=in0, scalar1=s1, scalar2=s2, op0=op0, op1=op1), r, w)

    def tt(self, eng, out, in0, in1, op, r=(), w=()):
        self.op(eng, lambda e: e.tensor_tensor(out=out, in0=in0, in1=in1, op=op), r, w)

    def stt(self, eng, out, in0, scalar, in1, op0, op1, r=(), w=()):
        self.op(eng, lambda e: e.scalar_tensor_tensor(out=out, in0=in0, scalar=scalar, in1=in1, op0=op0, op1=op1), r, w)

    def copy(self, eng, out, in_, r=(), w=()):
        if eng == 'act':
            self.op(eng, lambda e: e.copy(out=out, in_=in_), r, w)
        else:
            self.op(eng, lambda e: e.tensor_copy(out=out, in_=in_), r, w)

    def memset(self, eng, ap, val, w=()):
        self.op(eng, lambda e: e.memset(ap, val), (), w)

    def dma(self, eng, out, in_, r=(), w=(), key=None):
        self.op(eng, lambda e: e.dma_start(out=out, in_=in_), r, w, dma=True, key=key)

    def run(self):
        nc = self.nc
        ops = self.ops
        last_w = {}
        readers = {}
        deps = []
        for i, o in enumerate(ops):
            d = set()
            excl = [r for r in o['reads'] if r.startswith("PS:")]
            if excl:
                o['writes'] = tuple(o['writes']) + tuple(x for x in excl if x not in o['writes'])
            for r in o['reads']:
                if r in last_w:
                    d.add(last_w[r])
            for w in o['writes']:
                if w in last_w:
                    d.add(last_w[w])
                d.update(readers.get(w, ()))
            d.discard(i)
            for r in o['reads']:
                readers.setdefault(r, []).append(i)
            for w in o['writes']:
                last_w[w] = i
                readers[w] = []
            deps.append(d)
        eidx = []
        ecount = {}
        for o in ops:
            ecount[o['eng']] = ecount.get(o['eng'], 0) + 1
            eidx.append(ecount[o['eng']])

        def same_ok(i, d):
            o, od = ops[i], ops[d]
            if od['eng'] != o['eng'] or o['dma']:
                return False
            if o['eng'] == 'pe':
                return True
            return eidx[i] - eidx[d] > 6

        for i, o in enumerate(ops):
            for d in deps[i]:
                od = ops[d]
                if od['dma']:
                    continue
                if same_ok(i, d):
                    continue
                od['signal'] = True
        lastop = {}
        for o in ops:
            if not o['dma']:
                lastop[o['eng']] = o
        for o in lastop.values():
            o['signal'] = True
        cnt = {}
        dcnt = {}
        for o in ops:
            if o['dma']:
                dcnt[o['key']] = dcnt.get(o['key'], 0) + 16
                o['dval'] = dcnt[o['key']]
            elif o['signal']:
                cnt[o['eng']] = cnt.get(o['eng'], 0) + 1
                o['cval'] = cnt[o['eng']]
        engs = ('pe', 'act', 'dve', 'pool', 'sp')
        with ExitStack() as es:
            esem = {e: nc.alloc_semaphore(name=f"{self.name}_s_{e}") for e in engs if cnt.get(e)}
            dsem = {k: nc.alloc_semaphore(name=f"{self.name}_d_{j}") for j, k in enumerate(dcnt)}
            seen = {e: {} for e in engs}
            for i, o in enumerate(ops):
                w = {}
                for d in deps[i]:
                    od = ops[d]
                    if od['dma']:
                        s, v = ('d', od['key']), od['dval']
                    elif same_ok(i, d):
                        continue
                    else:
                        s, v = ('e', od['eng']), od['cval']
                    if seen[o['eng']].get(s, 0) >= v:
                        continue
                    w[s] = max(w.get(s, 0), v)
                for s, v in w.items():
                    seen[o['eng']][s] = v
                o['waits'] = [((dsem[s[1]] if s[0] == 'd' else esem[s[1]]), v) for s, v in w.items()]
            fence = [(dsem[k], v) for k, v in dcnt.items()] + [(esem[e], cnt[e]) for e in esem]
            block = es.enter_context(nc.Block())

            def make(ename):
                def body(eng):
                    for o in ops:
                        if o['eng'] != ename:
                            continue
                        for s, v in o['waits']:
                            eng.wait_ge(s, v)
                        ins = o['fn'](eng)
                        if o['dma']:
                            ins.then_inc(dsem[o['key']], 16)
                        elif o['signal']:
                            ins.then_inc(esem[ename], 1)
                    if ename == 'sp':
                        for s, v in fence:
                            eng.wait_ge(s, v)
                return body

            block.tensor(make('pe'))
            block.scalar(make('act'))
            block.vector(make('dve'))
            block.gpsimd(make('pool'))
            block.sync(make('sp'))
        nc.all_engine_barrier()
        nc.clear_and_free_semaphores(list(esem.values()) + list(dsem.values()))
        nc.all_engine_barrier()
        return len(ops)


class Rot:
    def __init__(self, items):
        self.items = items
        self.i = 0

    def next(self):
        r = self.items[self.i % len(self.items)]
        self.i += 1
        return r


_UID = [0]


def _sb(nc, es, name, shape, dt):
    _UID[0] += 1
    return es.enter_context(nc.sbuf_tensor(f"{name}_u{_UID[0]}", list(shape), dt))


def _ps(nc, es, name, shape, dt):
    _UID[0] += 1
    return es.enter_context(nc.psum_tensor(f"{name}_u{_UID[0]}", list(shape), dt))


def emit_norm_T(P, src, gamt, xin, hn, stat, junk, ptr, hnT_dst, identb, tag, evac_eng, hnT_res, keep=None):
    xt, xr = xin
    hnt, hr = hn
    stt_, sr = stat
    pt, pr = ptr
    P.dma('sp', xt[:], src, w=[xr], key=xr)
    P.act(junk[:], xt[:], AF.Square, accum=stt_[:, 0:1], r=[xr], w=[sr])
    P.ts('dve', stt_[:, 1:2], stt_[:, 0:1], 1.0 / D, EPS, ALU.mult, ALU.add, r=[sr], w=[sr])
    P.act(stt_[:, 2:3], stt_[:, 1:2], AF.Sqrt, r=[sr], w=[sr])
    P.op('dve', lambda e: e.reciprocal(out=stt_[:, 3:4], in_=stt_[:, 2:3]), [sr], [sr])
    P.stt('dve', hnt[:], xt[:], stt_[:, 3:4], gamt[:], ALU.mult, ALU.mult, r=[xr, sr, 'gam'], w=[hr])
    for c in range(8):
        P.tr(pt[:, c * 128:(c + 1) * 128], hnt[:, c * 128:(c + 1) * 128], identb[:], r=[hr, 'identb'], w=[pr])
    P.copy(evac_eng, hnT_dst, pt[:].rearrange("p (c t) -> p c t", c=8), r=[pr], w=[hnT_res])


def build(debug=False, upto=99):
    nc = bass.Bass("TRN2", target_bir_lowering=False)

    def din(name, shape, dt=F32):
        return nc.dram_tensor(name, list(shape), dt, kind="ExternalInput").ap()

    import os
    keep = os.environ.get("DBG_OUT", "").split(",")

    def dscr(name, shape, dt):
        ext = debug and (keep == [""] or name in keep)
        return nc.dram_tensor(name, list(shape), dt, kind=("ExternalOutput" if ext else "Internal")).ap()

    x = din("x", [T, D])
    gam = din("gam", [5, D])
    cst = din("cst", [128, NCST])
    sel = din("sel", [16, 2048])
    dn_win = din("dn_win", [128, 32, 8, 128])
    dn_wg = din("dn_wg", [128, 8, 16])
    dn_cw = din("dn_cw", [128, 24, 4])
    dn_vec = din("dn_vec", [16])
    dn_ong = din("dn_ong", [128, 1])
    dn_wout = din("dn_wout", [128, 8, 1024])
    mb_wqk = din("mb_wqk", [128, 16, 8, 128])
    mb_wv = din("mb_wv", [128, 8, 1024])
    mb_wout = din("mb_wout", [128, 8, 1024])
    wup = din("wup", [2, 128, 32, 8, 128])
    wdn = din("wdn", [2, 128, 32, 1024])
    out = nc.dram_tensor("out", [T, D], F32, kind="ExternalOutput").ap()

    qT_s = dscr("qT_s", [128, 8, T], F32)
    kT_s = dscr("kT_s", [128, 8, T], F32)
    vT_s = dscr("vT_s", [128, 8, T], F32)
    szT_s = dscr("szT_s", [128, 8, T], BF16)
    graw_s = dscr("graw_s", [128, NT, 16], F32)
    ss_s = dscr("ss_s", [128, NT, 16], F32)
    oT_s = dscr("oT_s", [128, 8, T], BF16)
    h1_s = dscr("h1_s", [T, D], F32)
    h2_s = dscr("h2_s", [T, D], F32)
    h3_s = dscr("h3_s", [T, D], F32)
    mq_s = dscr("mq_s", [128, 8, T], BF16)
    mk_s = dscr("mk_s", [128, 8, T], BF16)
    mv_s = dscr("mv_s", [T, 8, 129], BF16)
    msel_s = dscr("msel_s", [8, 16, T], BF16)
    o2T_s = dscr("o2T_s", [128, 8, T], BF16)

    only = os.environ.get('ONLY')
    run_ = lambda n: upto >= n and (only is None or int(only) == n)
    if run_(1) and not os.environ.get('SKIP1'):
        phase_gdn_proj(nc, x, gam, cst, dn_win, dn_wg, dn_cw, qT_s, kT_s, vT_s, szT_s, graw_s, ss_s)
    if run_(2):
        phase_gdn_core(nc, x, cst, sel, dn_vec, dn_ong, dn_wout, qT_s, kT_s, vT_s, szT_s, graw_s, ss_s, h1_s)
    if run_(3):
        phase_mlp(nc, "m0", h1_s, h2_s, gam[1], None, cst, wup[0], wdn[0])
    if run_(4):
        phase_moba_proj(nc, h2_s, gam, cst, mb_wqk, mb_wv, mq_s, mk_s, mv_s, msel_s)
    if run_(5):
        phase_moba_attn(nc, cst, sel, mq_s, mk_s, mv_s, msel_s, o2T_s)
    if run_(6):
        phase_outproj(nc, o2T_s, mb_wout, h2_s, h3_s)
    if run_(7):
        phase_mlp(nc, "m1", h3_s, out, gam[3], gam[4], cst, wup[1], wdn[1])
    return nc


def phase_gdn_proj(nc, x, gam, cst, dn_win, dn_wg, dn_cw, qT_s, kT_s, vT_s, szT_s, graw_s, ss_s):
    with ExitStack() as es:
        sb = lambda n, s, d: _sb(nc, es, n, s, d)
        W = sb("W", [128, 32, 8, 128], BF16)
        Wg = sb("Wg", [128, 8, 16], BF16)
        gamt = sb("gamt", [128, D], F32)
        cw = sb("cw", [128, 24, 4], F32)
        cs = sb("cs", [128, 640], F32)
        identb = sb("identb", [128, 128], BF16)
        halo = sb("halo", [128, 24, 3], F32)
        graw = sb("graw", [128, NT, 16], F32)
        ssraw = sb("ssraw", [128, NT, 16], F32)
        xin = Rot([(sb(f"xin{i}", [128, D], F32), f"xin{i}") for i in range(2)])
        hn = Rot([(sb(f"hn{i}", [128, D], BF16), f"hn{i}") for i in range(2)])
        stat = Rot([(sb(f"stat{i}", [128, 4], F32), f"stat{i}") for i in range(2)])
        junk = sb("junk", [128, D], BF16)
        hnT = Rot([(sb(f"hnT{i}", [128, 8, 512], BF16), f"hnT{i}") for i in range(2)])
        praw = Rot([(sb(f"praw{i}", [128, 515], F32), f"praw{i}") for i in range(3)])
        yv = Rot([(sb(f"yv{i}", [128, 512], F32), f"yv{i}") for i in range(3)])
        sy = Rot([(sb(f"sy{i}", [128, 512], F32), f"sy{i}") for i in range(3)])
        sq = Rot([(sb(f"sq{i}", [128, 512], F32), f"sq{i}") for i in range(2)])
        szb = Rot([(sb(f"szb{i}", [128, 512], BF16), f"szb{i}") for i in range(2)])
        ptr = Rot([(_ps(nc, es, f"ptr{i}", [128, 1024], BF16), f"PS:ptr{i}") for i in range(2)])
        pmm = Rot([(_ps(nc, es, f"pmm{i}", [128, 512], F32), f"PS:pmm{i}") for i in range(4)])
        pss = _ps(nc, es, "pss", [128, 512], F32)[:, 0:64].rearrange("p (j c) -> p j c", j=4)
        pg = _ps(nc, es, "pg", [128, 512], F32)[:, 0:64].rearrange("p (j c) -> p j c", j=4)

        P = Phase(nc, "gp")
        P.dma('sp', cs[:], cst[:, 0:640], w=['cs'], key='cs')
        P.dma('sp', gamt[:], gam[0].partition_broadcast(128), w=['gam'], key='gam')
        P.dma('sp', cw[:], dn_cw, w=['cw'], key='cw')
        for i in range(8):
            P.dma('pool', W[:, 4 * i:4 * i + 4], dn_win[:, 4 * i:4 * i + 4], w=[f'W{i}'], key=f'W{i}')
        P.dma('pool', Wg[:], dn_wg, w=['Wg'], key='Wg')
        P.copy('dve', identb[:], cs[:, C_ID:C_ID + 128], r=['cs'], w=['identb'])
        P.memset('pool', halo[:], 0.0, w=['halo%d' % c_ for c_ in range(24)])
        ones_col = cs[:, C_ONE:C_ONE + 1]
        for tb in range(8):
            hT, hTr = hnT.next()
            for j in range(4):
                t = tb * 4 + j
                emit_norm_T(P, x[t * 128:(t + 1) * 128, :], gamt, xin.next(), hn.next(), stat.next(), junk,
                            ptr.next(), hT[:, :, j * 128:(j + 1) * 128], identb, "gp", 'act' if j % 2 else 'dve',
                            f"{hTr}_{j}")
            hres = [f"{hTr}_{j}" for j in range(4)]
            for j in range(4):
                for c in range(8):
                    P.mm(pg[:, j, :], hT[:, c, j * 128:(j + 1) * 128], Wg[:, c, :], start=(c == 0), stop=(c == 7),
                         r=[hres[j], 'Wg'], w=['PS:pg'])
            P.copy('act', graw[:, tb * 4:tb * 4 + 4, :], pg, r=['PS:pg'], w=['graw'])
            for cc in range(32):
                pm, pmr = pmm.next()
                for c in range(8):
                    P.mm(pm[:], W[:, cc, c, :], hT[:, c, :], start=(c == 0), stop=(c == 7),
                         r=hres + [f'W{cc // 4}'], w=[pmr])
                h = cc % 8
                sl = slice(tb * 512, (tb + 1) * 512)
                if cc < 24:
                    pr_, prr = praw.next()
                    y, yr = yv.next()
                    s, sr = sy.next()
                    ce = 'dve'
                    P.copy('act', pr_[:, 3:515], pm[:], r=[pmr], w=[prr])
                    P.copy(ce, pr_[:, 0:3], halo[:, cc, :], r=['halo%d' % cc], w=[prr])
                    P.copy(ce, halo[:, cc, :], pr_[:, 512:515], r=[prr], w=['halo%d' % cc])
                    P.ts(ce, y[:], pr_[:, 0:512], cw[:, cc, 0:1], None, ALU.mult, r=[prr, 'cw'], w=[yr])
                    for k in range(1, 4):
                        P.stt(ce, y[:], pr_[:, k:k + 512], cw[:, cc, k:k + 1], y[:], ALU.mult, ALU.add,
                              r=[prr, 'cw', yr], w=[yr])
                    P.act(s[:], y[:], AF.Silu, r=[yr], w=[sr])
                    dst = (qT_s, kT_s, vT_s)[cc // 8]
                    P.dma('sp', dst[:, h, sl], s[:], r=[sr], w=[], key=sr)
                    if cc < 16:
                        q2, q2r = sq.next()
                        P.tt('pool', q2[:], s[:], s[:], ALU.mult, r=[sr], w=[q2r])
                        for j in range(4):
                            P.mm(pss[:, j, cc:cc + 1], q2[:, j * 128:(j + 1) * 128], ones_col, r=[q2r, 'cs'], w=['PS:pss'])
                        if cc == 15:
                            P.copy('dve', ssraw[:, tb * 4:tb * 4 + 4, :], pss, r=['PS:pss'], w=['ssraw'])
                else:
                    zb, zr = szb.next()
                    P.act(zb[:], pm[:], AF.Silu, r=[pmr], w=[zr])
                    P.dma('sp', szT_s[:, h, sl], zb[:], r=[zr], w=[], key=zr)
        P.dma('sp', graw_s, graw[:], r=['graw'], key='graw_o')
        P.dma('sp', ss_s, ssraw[:], r=['ssraw'], key='ss_o')
        P.run()


def phase_gdn_core(nc, x, cst, sel, dn_vec, dn_ong, dn_wout, qT_s, kT_s, vT_s, szT_s, graw_s, ss_s, h1_s):
    with ExitStack() as es:
        sb = lambda n, s, d: _sb(nc, es, n, s, d)
        cs = sb("cs", [128, 640], F32)
        identb = sb("identb", [128, 128], BF16)
        wout = sb("wout", [128, 8, 1024], BF16)
        ong = sb("ong", [128, 1], F32)
        vecb = sb("vecb", [128, 16], F32)
        graw = sb("graw", [128, NT, 16], F32)
        ssr = sb("ssr", [128, NT, 16], F32)
        G = {n: sb("g_" + n, [128, NT, 8], F32) for n in
             ("l1", "g", "gc", "gl", "lrq", "lrk", "bj", "ckbg", "ckdec", "cvb", "co1", "egt", "tmp", "tmp2")}
        rsrc = sb("rsrc", [128, NT, 16], F32)
        S = sb("S", [128, 8, 128], F32)
        qt_ = Rot([(sb(f"q{i}", [128, 8, 128], F32), f"q{i}") for i in range(2)])
        kt_ = Rot([(sb(f"k{i}", [128, 8, 128], F32), f"k{i}") for i in range(2)])
        vt_ = Rot([(sb(f"v{i}", [128, 8, 128], F32), f"v{i}") for i in range(2)])
        zt_ = Rot([(sb(f"z{i}", [128, 8, 128], BF16), f"z{i}") for i in range(2)])
        xt_ = Rot([(sb(f"x{i}", [128, D], F32), f"x{i}") for i in range(2)])
        ho_ = Rot([(sb(f"ho{i}", [128, D], F32), f"ho{i}") for i in range(2)])
        oTg = Rot([(sb(f"oTg{i}", [128, 8, 128], BF16), f"oTg{i}") for i in range(2)])
        names = ("kbg", "kdec", "vb", "DmU", "DmB", "M", "attnT", "A", "PA0", "PA1", "PM0", "PM1", "N0", "N1",
                 "nwT", "vnew", "O2", "o", "on", "dg1", "dg2", "usb", "ktm")
        Wk = {n: [sb(f"w_{n}{h}", [128, 128], F32) for h in range(8)] for n in names}
        ojunk = sb("ojunk", [128, 128], F32)
        ost = sb("ost", [128, 8, 4], F32)
        banks = [_ps(nc, es, f"bk{i}", [128, 512], F32) for i in range(8)]
        pbanks = Rot([(banks[i], f"PS:bk{i}") for i in range(6)])
        pbig = Rot([(banks[6 + i], f"PS:pb{i}") for i in range(2)])

        P = Phase(nc, "gc")
        P.dma('sp', cs[:], cst[:, 0:640], w=['cs'], key='cs')
        P.dma('sp', ong[:], dn_ong, w=['ong'], key='ong')
        P.dma('sp', vecb[:], dn_vec.partition_broadcast(128), w=['vecb'], key='vecb')
        P.dma('sp', graw[:], graw_s, w=['graw'], key='graw')
        P.dma('sp', ssr[:], ss_s, w=['ssr'], key='ssr')
        P.dma('pool', wout[:], dn_wout, w=['wout'], key='wout')
        P.copy('dve', identb[:], cs[:, C_ID:C_ID + 128], r=['cs'], w=['identb'])
        P.memset('pool', S[:], 0.0, w=[f'S{h}' for h in range(8)])
        identf = cs[:, C_ID:C_ID + 128]
        maskU = cs[:, C_MU:C_MU + 128]
        maskSU = cs[:, C_MSU:C_MSU + 128]
        tri = cs[:, C_TRI:C_TRI + 128]
        ones = cs[:, C_ONE:C_ONE + 128]

        b_raw = graw[:, :, 0:8]
        a_raw = graw[:, :, 8:16]
        gr = ['graw', 'vecb', 'ssr', 'cs']
        P.act(G["tmp"][:], b_raw, AF.Exp, scale=-1.0, r=gr, w=['g_tmp'])
        P.act(G["l1"][:], G["tmp"][:], AF.Ln, bias=1.0, r=['g_tmp'], w=['g_l1'])
        P.act(G["cvb"][:], G["l1"][:], AF.Exp, scale=-1.0, r=['g_l1'], w=['g_cvb'])
        for t in range(NT):
            P.tt('dve', G["tmp2"][:, t, :], a_raw[:, t, :], vecb[:, 8:16], ALU.add, r=gr, w=['g_tmp2'])
        P.act(G["tmp"][:], G["tmp2"][:], AF.Exp, r=['g_tmp2'], w=['g_tmp'])
        P.act(G["tmp2"][:], G["tmp"][:], AF.Ln, bias=1.0, r=['g_tmp'], w=['g_tmp2'])
        P.act(vecb[:, 0:8], vecb[:, 0:8], AF.Exp, r=['vecb'], w=['vecb2'])
        for t in range(NT):
            P.stt('dve', G["g"][:, t, :], G["tmp2"][:, t, :], -1.0, vecb[:, 0:8], ALU.mult, ALU.mult,
                  r=['g_tmp2', 'vecb2'], w=['g_g'])
        g2 = G["g"][:].rearrange("p t h -> p (t h)")
        pb, pbr = pbig.next()
        P.mm(pb[:, 0:256], tri, g2, r=['g_g', 'cs'], w=[pbr])
        P.copy('dve', G["gc"][:].rearrange("p t h -> p (t h)"), pb[:, 0:256], r=[pbr], w=['g_gc'])
        pb2, pb2r = pbig.next()
        P.mm(pb2[:, 0:256], ones, g2, r=['g_g', 'cs'], w=[pb2r])
        P.copy('dve', G["gl"][:].rearrange("p t h -> p (t h)"), pb2[:, 0:256], r=[pb2r], w=['g_gl'])
        P.ts('dve', G["tmp"][:], ssr[:, :, 0:8], EPS, None, ALU.add, r=gr, w=['g_tmp'])
        P.act(G["tmp"][:], G["tmp"][:], AF.Ln, r=['g_tmp'], w=['g_tmp'])
        P.ts('dve', G["lrq"][:], G["tmp"][:], -0.5, float(np.log(128.0 ** -0.5)), ALU.mult, ALU.add, r=['g_tmp'], w=['g_lrq'])
        P.ts('dve', G["tmp"][:], ssr[:, :, 8:16], EPS, None, ALU.add, r=gr + ['g_lrq'], w=['g_tmp'])
        P.act(G["tmp"][:], G["tmp"][:], AF.Ln, r=['g_tmp'], w=['g_tmp'])
        P.ts('dve', G["lrk"][:], G["tmp"][:], -0.5, None, ALU.mult, r=['g_tmp'], w=['g_lrk'])
        P.tt('dve', rsrc[:, :, 0:8], G["gc"][:], G["lrq"][:], ALU.add, r=['g_gc', 'g_lrq'], w=['rsrc'])
        P.tt('dve', G["tmp"][:], G["gc"][:], G["l1"][:], ALU.subtract, r=['g_gc', 'g_l1', 'g_lrk'], w=['g_tmp'])
        P.tt('dve', rsrc[:, :, 8:16], G["tmp"][:], G["lrk"][:], ALU.add, r=['g_tmp', 'g_lrk'], w=['rsrc'])
        P.tt('dve', G["bj"][:], G["lrk"][:], G["gc"][:], ALU.subtract, r=['g_gc', 'g_lrk'], w=['g_bj'])
        P.act(G["ckbg"][:], rsrc[:, :, 8:16], AF.Exp, r=['rsrc'], w=['g_ckbg'])
        P.act(G["co1"][:], rsrc[:, :, 0:8], AF.Exp, r=['rsrc'], w=['g_co1'])
        P.tt('dve', G["tmp2"][:], G["gl"][:], G["bj"][:], ALU.add, r=['g_gl', 'g_bj', 'g_g'], w=['g_tmp2'])
        P.act(G["ckdec"][:], G["tmp2"][:], AF.Exp, r=['g_tmp2'], w=['g_ckdec'])
        P.act(G["egt"][:], G["gl"][:], AF.Exp, r=['g_gl'], w=['g_egt'])
        gall = ['g_ckbg', 'g_co1', 'g_ckdec', 'g_egt', 'g_cvb', 'g_bj', 'rsrc']

        def loads(t):
            sl = slice(t * 128, (t + 1) * 128)
            q, qr = qt_.next(); k, kr = kt_.next(); v, vr = vt_.next(); z, zr = zt_.next(); xx, xr = xt_.next()
            P.dma('sp', q[:], qT_s[:, :, sl], w=[qr], key=qr)
            P.dma('sp', k[:], kT_s[:, :, sl], w=[kr], key=kr)
            P.dma('sp', v[:], vT_s[:, :, sl], w=[vr], key=vr)
            P.dma('sp', z[:], szT_s[:, :, sl], w=[zr], key=zr)
            P.dma('sp', xx[:], x[sl, :], w=[xr], key=xr)
            return (q, qr, k, kr, v, vr, z, zr, xx, xr)

        nxt = loads(0)
        import os
        HS = list(range(int(os.environ.get('GC_HEADS', 8))))
        NTL = int(os.environ.get('GC_TILES', NT))
        STG = int(os.environ.get('GC_STAGE', 99))
        groups = [HS[i:i + 4] for i in range(0, len(HS), 4)]
        rs = lambda n, h: f"w_{n}{h}"

        def stage(mmfn, consfns):
            for grp in groups:
                bk, bkr = pbanks.next()
                for i, h in enumerate(grp):
                    mmfn(h, bk[:, i * 128:(i + 1) * 128], bkr)
                for cf in consfns:
                    for i, h in enumerate(grp):
                        cf(h, bk[:, i * 128:(i + 1) * 128], bkr)

        for t in range(NTL):
            q, qr, k, kr, v, vr, z, zr, xx, xr = nxt
            if t + 1 < NTL:
                nxt = loads(t + 1)
            if STG < 1:
                continue
            stage(lambda h, pq, br: P.tr(pq, k[:, h, :], identf, r=[kr, 'cs'], w=[br]),
                  [lambda h, pq, br: P.copy('act', Wk["ktm"][h][:], pq, r=[br], w=[rs("ktm", h)])])
            for h in HS:
                P.ts('pool', Wk["kbg"][h][:], Wk["ktm"][h][:], G["ckbg"][:, t, h:h + 1], None, ALU.mult,
                     r=[rs("ktm", h)] + gall, w=[rs("kbg", h)])
                P.ts('pool', Wk["kdec"][h][:], Wk["ktm"][h][:], G["ckdec"][:, t, h:h + 1], None, ALU.mult,
                     r=[rs("ktm", h)] + gall, w=[rs("kdec", h)])
            stage(lambda h, pq, br: P.tr(pq, v[:, h, :], identf, r=[vr, 'cs'], w=[br]),
                  [lambda h, pq, br: P.act(Wk["vb"][h][:], pq, AF.Copy, scale=G["cvb"][:, t, h:h + 1], r=[br] + gall,
                                           w=[rs("vb", h)])])
            if STG < 2:
                continue
            for dgn, col0, msk, dst in (("dg1", 0, maskU, "DmU"), ("dg2", 8, maskSU, "DmB")):
                for h in HS:
                    P.ts('pool', Wk[dgn][h][:], identf, rsrc[:, t, col0 + h:col0 + h + 1], None, ALU.mult,
                         r=['cs', 'rsrc'], w=[rs(dgn, h)])
                stage(lambda h, pq, br, dgn=dgn: P.mm(pq, ones, Wk[dgn][h][:], r=['cs', rs(dgn, h)], w=[br]),
                      [lambda h, pq, br, dst=dst, msk=msk: P.stt('dve', Wk[dst][h][:], pq, G["bj"][:, t, h:h + 1], msk,
                                                               ALU.add, ALU.add, r=[br, 'cs'] + gall, w=[rs(dst, h)])])
                for h in HS:
                    P.act(Wk[dst][h][:], Wk[dst][h][:], AF.Exp, r=[rs(dst, h)], w=[rs(dst, h)])
            stage(lambda h, pq, br: P.mm(pq, k[:, h, :], k[:, h, :], r=[kr], w=[br]),
                  [lambda h, pq, br: P.tt('dve', Wk["M"][h][:], pq, Wk["DmB"][h][:], ALU.mult, r=[br, rs("DmB", h)],
                                          w=[rs("M", h)])])
            stage(lambda h, pq, br: P.mm(pq, k[:, h, :], q[:, h, :], r=[kr, qr], w=[br]),
                  [lambda h, pq, br: P.tt('dve', Wk["attnT"][h][:], pq, Wk["DmU"][h][:], ALU.mult, r=[br, rs("DmU", h)],
                                          w=[rs("attnT", h)])])
            if STG < 3:
                continue
            stage(lambda h, pq, br: P.tr(pq, Wk["M"][h][:], identf, r=[rs("M", h), 'cs'], w=[br]),
                  [lambda h, pq, br: P.copy('act', Wk["A"][h][:], pq, r=[br], w=[rs("A", h)])])
            for h in HS:
                P.tt('pool', Wk["N0"][h][:], identf, Wk["M"][h][:], ALU.subtract, r=['cs', rs("M", h)], w=[rs("N0", h)])
            if STG < 4:
                continue
            PAc = {h: (Wk["A"][h], rs("A", h)) for h in HS}
            PMc = {h: (Wk["M"][h], rs("M", h)) for h in HS}
            Nc = {h: (Wk["N0"][h], rs("N0", h)) for h in HS}
            for lvl in range(6):
                last = (lvl == 5)
                npa = {h: (Wk["PA%d" % (lvl % 2)][h], rs("PA%d" % (lvl % 2), h)) for h in HS}
                npm = {h: (Wk["PM%d" % (lvl % 2)][h], rs("PM%d" % (lvl % 2), h)) for h in HS}
                nnn = {h: (Wk["N%d" % ((lvl + 1) % 2)][h], rs("N%d" % ((lvl + 1) % 2), h)) for h in HS}
                stage(lambda h, pq, br: P.mm(pq, PMc[h][0][:], PAc[h][0][:], r=[PMc[h][1], PAc[h][1]], w=[br]),
                      [lambda h, pq, br: P.copy('act', npa[h][0][:], pq, r=[br], w=[npa[h][1]])])
                if not last:
                    stage(lambda h, pq, br: P.mm(pq, PAc[h][0][:], PMc[h][0][:], r=[PMc[h][1], PAc[h][1]], w=[br]),
                          [lambda h, pq, br: P.copy('dve', npm[h][0][:], pq, r=[br], w=[npm[h][1]])])
                stage(lambda h, pq, br: P.mm(pq, npa[h][0][:], Nc[h][0][:], r=[npa[h][1], Nc[h][1]], w=[br]),
                      [lambda h, pq, br: P.tt('dve', nnn[h][0][:], pq, Nc[h][0][:], ALU.add, r=[br, Nc[h][1]],
                                              w=[nnn[h][1]])])
                PAc = npa
                if not last:
                    PMc = npm
                Nc = nnn
            if STG < 5:
                continue
            stage(lambda h, pq, br: P.mm(pq, Wk["kbg"][h][:], Nc[h][0][:], r=[rs("kbg", h), Nc[h][1]], w=[br]),
                  [lambda h, pq, br: P.ts('dve', Wk["nwT"][h][:], pq, -1.0, None, ALU.mult, r=[br], w=[rs("nwT", h)])])
            if STG < 6:
                continue
            stage(lambda h, pq, br: P.mm(pq, Nc[h][0][:], Wk["vb"][h][:], r=[Nc[h][1], rs("vb", h)], w=[br]),
                  [lambda h, pq, br: P.copy('act', Wk["usb"][h][:], pq, r=[br], w=[rs("usb", h)])])
            stage(lambda h, pq, br: P.mm(pq, Wk["nwT"][h][:], S[:, h, :], r=[rs("nwT", h), f'S{h}'], w=[br]),
                  [lambda h, pq, br: P.tt('dve', Wk["vnew"][h][:], pq, Wk["usb"][h][:], ALU.add, r=[br, rs("usb", h)],
                                          w=[rs("vnew", h)])])
            stage(lambda h, pq, br: P.mm(pq, Wk["attnT"][h][:], Wk["vnew"][h][:], r=[rs("attnT", h), rs("vnew", h)], w=[br]),
                  [lambda h, pq, br: P.copy('act', Wk["O2"][h][:], pq, r=[br], w=[rs("O2", h)])])
            stage(lambda h, pq, br: P.mm(pq, q[:, h, :], S[:, h, :], r=[qr, f'S{h}'], w=[br]),
                  [lambda h, pq, br: P.stt('dve', Wk["o"][h][:], pq, G["co1"][:, t, h:h + 1], Wk["O2"][h][:], ALU.mult,
                                           ALU.add, r=[br, rs("O2", h)] + gall, w=[rs("o", h)])])
            stage(lambda h, pq, br: P.mm(pq, Wk["kdec"][h][:], Wk["vnew"][h][:], r=[rs("kdec", h), rs("vnew", h)], w=[br]),
                  [lambda h, pq, br: P.stt('dve', S[:, h, :], S[:, h, :], G["egt"][:, t, h:h + 1], pq, ALU.mult, ALU.add,
                                           r=[br, f'S{h}'] + gall, w=[f'S{h}'])])
            for h in HS:
                P.act(ojunk[:], Wk["o"][h][:], AF.Square, accum=ost[:, h, 0:1], r=[rs("o", h)], w=['ost'])
            if STG < 7:
                continue
            P.ts('dve', ost[:, :, 1:2], ost[:, :, 0:1], 1.0 / 128, EPS, ALU.mult, ALU.add, r=['ost'], w=['ost'])
            P.act(ost[:, :, 2:3], ost[:, :, 1:2], AF.Sqrt, r=['ost'], w=['ost'])
            P.op('dve', lambda e: e.reciprocal(out=ost[:, :, 3:4], in_=ost[:, :, 2:3]), ['ost'], ['ost'])
            og, ogr = oTg.next()
            for h in HS:
                P.ts('pool', Wk["on"][h][:], Wk["o"][h][:], ost[:, h, 3:4], None, ALU.mult, r=[rs("o", h), 'ost'], w=[rs("on", h)])
            stage(lambda h, pq, br: P.tr(pq, Wk["on"][h][:], identf, r=[rs("on", h), 'cs'], w=[br]),
                  [lambda h, pq, br: P.stt('dve', og[:, h, :], pq, ong[:, 0:1], z[:, h, :], ALU.mult, ALU.mult,
                                           r=[br, 'ong', zr], w=[ogr])])
            if STG < 8:
                continue
            ho, hor = ho_.next()
            for half in range(2):
                pb, pbr = pbig.next()
                for h in HS:
                    P.mm(pb[:], og[:, h, :], wout[:, h, half * 512:(half + 1) * 512], start=(h == 0), stop=(h == HS[-1]),
                         r=[ogr, 'wout'], w=[pbr])
                P.tt('dve', ho[:, half * 512:(half + 1) * 512], pb[:], xx[:, half * 512:(half + 1) * 512], ALU.add,
                     r=[pbr, xr], w=[hor])
            P.dma('sp', h1_s[t * 128:(t + 1) * 128, :], ho[:], r=[hor], key=hor)
        P.run()


def phase_mlp(nc, name, h_in, h_out, gam_mlp, gam_final, cst, wup_l, wdn_l):
    with ExitStack() as es:
        sb = lambda n, s, d: _sb(nc, es, n, s, d)
        Wu = sb("Wu", [128, 32, 8, 128], BF16)
        Wd = sb("Wd", [128, 32, 1024], BF16)
        gamt = sb("gamt", [128, D], F32)
        gamf = sb("gamf", [128, D], F32) if gam_final is not None else None
        idf = sb("idf", [128, 128], F32)
        identb = sb("identb", [128, 128], BF16)
        a1T = sb("a1T", [128, 32, 256], BF16)
        hmid = Rot([(sb(f"hmid{i}", [128, D], F32), f"hmid{i}") for i in range(4)])
        hn = Rot([(sb(f"hn{i}", [128, D], BF16), f"hn{i}") for i in range(2)])
        stat = Rot([(sb(f"stat{i}", [128, 4], F32), f"stat{i}") for i in range(2)])
        fstat = Rot([(sb(f"fstat{i}", [128, 4], F32), f"fstat{i}") for i in range(2)])
        junk = sb("junk", [128, D], BF16)
        hnT = Rot([(sb(f"hnT{i}", [128, 8, 256], BF16), f"hnT{i}") for i in range(2)])
        rl = Rot([(sb(f"rl{i}", [128, 256], F32), f"rl{i}") for i in range(3)])
        ost = Rot([(sb(f"ost{i}", [128, D], F32), f"ost{i}") for i in range(2)])
        ptr = Rot([(_ps(nc, es, f"ptr{i}", [128, 1024], BF16), f"PS:ptr{i}") for i in range(2)])
        pup = Rot([(_ps(nc, es, f"pup{i}", [128, 512], F32), f"PS:pup{i}") for i in range(3)])
        pdn = Rot([(_ps(nc, es, f"pdn{i}", [128, 512], F32), f"PS:pdn{i}") for i in range(3)])

        P = Phase(nc, name)
        P.dma('sp', idf[:], cst[:, C_ID:C_ID + 128], w=['idf'], key='idf')
        P.dma('sp', gamt[:], gam_mlp.partition_broadcast(128), w=['gam'], key='gam')
        if gamf is not None:
            P.dma('sp', gamf[:], gam_final.partition_broadcast(128), w=['gamf'], key='gamf')
        for i in range(8):
            P.dma('pool', Wu[:, 4 * i:4 * i + 4], wup_l[:, 4 * i:4 * i + 4], w=[f'Wu{i}'], key=f'Wu{i}')
        for i in range(8):
            P.dma('pool', Wd[:, 4 * i:4 * i + 4], wdn_l[:, 4 * i:4 * i + 4], w=[f'Wd{i}'], key=f'Wd{i}')
        P.copy('dve', identb[:], idf[:], r=['idf'], w=['identb'])
        for tb in range(16):
            hT, hTr = hnT.next()
            hm = []
            for j in range(2):
                t = tb * 2 + j
                xm = hmid.next()
                hm.append(xm)
                emit_norm_T(P, h_in[t * 128:(t + 1) * 128, :], gamt, xm, hn.next(), stat.next(), junk,
                            ptr.next(), hT[:, :, j * 128:(j + 1) * 128], identb, name, 'act' if j % 2 else 'dve',
                            f"{hTr}_{j}")
            hres = [f"{hTr}_{j}" for j in range(2)]
            for f in range(32):
                pu, pur = pup.next()
                for c in range(8):
                    P.mm(pu[:, 0:256], Wu[:, f, c, :], hT[:, c, :], start=(c == 0), stop=(c == 7),
                         r=hres + [f'Wu{f // 4}'], w=[pur])
                r_, rr = rl.next()
                P.act(r_[:], pu[:, 0:256], AF.Relu, r=[pur], w=[rr])
                P.tt('pool', a1T[:, f, :], r_[:], r_[:], ALU.mult, r=[rr], w=[f'a1T{f}'])
            ares = [f'a1T{f}' for f in range(32)]
            for j in range(2):
                t = tb * 2 + j
                xm, xmr = hm[j]
                o_, orr = ost.next()
                for half in range(2):
                    pd, pdr = pdn.next()
                    for f in range(32):
                        P.mm(pd[:], a1T[:, f, j * 128:(j + 1) * 128], Wd[:, f, half * 512:(half + 1) * 512],
                             start=(f == 0), stop=(f == 31), r=[f'a1T{f}', f'Wd{f // 4}'], w=[pdr])
                    P.tt('dve', o_[:, half * 512:(half + 1) * 512], pd[:], xm[:, half * 512:(half + 1) * 512], ALU.add,
                         r=[pdr, xmr], w=[orr])
                if gamf is not None:
                    fs, fsr = fstat.next()
                    P.act(junk[:], o_[:], AF.Square, accum=fs[:, 0:1], r=[orr], w=[fsr])
                    P.ts('dve', fs[:, 1:2], fs[:, 0:1], 1.0 / D, EPS, ALU.mult, ALU.add, r=[fsr], w=[fsr])
                    P.act(fs[:, 2:3], fs[:, 1:2], AF.Sqrt, r=[fsr], w=[fsr])
                    P.op('dve', (lambda fs=fs: (lambda e: e.reciprocal(out=fs[:, 3:4], in_=fs[:, 2:3])))(), [fsr], [fsr])
                    P.stt('dve', o_[:], o_[:], fs[:, 3:4], gamf[:], ALU.mult, ALU.mult, r=[orr, fsr, 'gamf'], w=[orr])
                P.dma('sp', h_out[t * 128:(t + 1) * 128, :], o_[:], r=[orr], key=orr)
        P.run()


def phase_moba_proj(nc, h_in, gam, cst, mb_wqk, mb_wv, mq_s, mk_s, mv_s, msel_s):
    with ExitStack() as es:
        sb = lambda n, s, d: _sb(nc, es, n, s, d)
        W = sb("W", [128, 16, 8, 128], BF16)
        Wv = sb("Wv", [128, 8, 1024], BF16)
        gamt = sb("gamt", [128, D], F32)
        cs = sb("cs", [128, NCST], F32)
        identb = sb("identb", [128, 128], BF16)
        kms = sb("kms", [128, 8, 16], F32)
        xin = Rot([(sb(f"xin{i}", [128, D], F32), f"xin{i}") for i in range(2)])
        hn = Rot([(sb(f"hn{i}", [128, D], BF16), f"hn{i}") for i in range(2)])
        stat = Rot([(sb(f"stat{i}", [128, 4], F32), f"stat{i}") for i in range(2)])
        junk = sb("junk", [128, D], BF16)
        hnT = Rot([(sb(f"hnT{i}", [128, 8, 512], BF16), f"hnT{i}") for i in range(2)])
        kblk = Rot([(sb(f"kblk{i}", [128, 8, 512], BF16), f"kblk{i}") for i in range(2)])
        qblk = Rot([(sb(f"qblk{i}", [128, 8, 512], BF16), f"qblk{i}") for i in range(2)])
        q32 = Rot([(sb(f"q32{i}", [128, 512], F32), f"q32{i}") for i in range(2)])
        selb = Rot([(sb(f"selb{i}", [128, 4, 8, 16], F32), f"selb{i}") for i in range(2)])
        selTb = Rot([(sb(f"selTb{i}", [128, 512], BF16), f"selTb{i}") for i in range(2)])
        gm = Rot([(sb(f"gm{i}", [128, 16], F32), f"gm{i}") for i in range(4)])
        t8 = Rot([(sb(f"t8{i}", [128, 8], F32), f"t8{i}") for i in range(4)])
        vaug = Rot([(sb(f"vaug{i}", [128, 8, 129], BF16), f"vaug{i}") for i in range(2)])
        ptr = Rot([(_ps(nc, es, f"ptr{i}", [128, 1024], BF16), f"PS:ptr{i}") for i in range(2)])
        pmm = Rot([(_ps(nc, es, f"pmm{i}", [128, 512], F32), f"PS:pmm{i}") for i in range(3)])
        pgt = _ps(nc, es, "pgt", [128, 512], F32)
        pst = _ps(nc, es, "pst", [128, 512], F32)

        P = Phase(nc, "mp")
        P.dma('sp', cs[:], cst, w=['cs'], key='cs')
        P.dma('sp', gamt[:], gam[2].partition_broadcast(128), w=['gam'], key='gam')
        for i in range(4):
            P.dma('pool', W[:, 4 * i:4 * i + 4], mb_wqk[:, 4 * i:4 * i + 4], w=[f'W{i}'], key=f'W{i}')
        P.dma('pool', Wv[:], mb_wv, w=['Wv'], key='Wv')
        P.copy('dve', identb[:], cs[:, C_ID:C_ID + 128], r=['cs'], w=['identb'])
        P.memset('pool', kms[:], 0.0, w=['kms'])
        for i in range(2):
            va, var = vaug.next()
            P.memset('pool', va[:], 1.0, w=[var])
        identf = cs[:, C_ID:C_ID + 128]
        scale = 128.0 ** -0.5
        for tb in range(8):
            hT, hTr = hnT.next()
            for j in range(4):
                t = tb * 4 + j
                emit_norm_T(P, h_in[t * 128:(t + 1) * 128, :], gamt, xin.next(), hn.next(), stat.next(), junk,
                            ptr.next(), hT[:, :, j * 128:(j + 1) * 128], identb, "mp", 'act' if j % 2 else 'dve',
                            f"{hTr}_{j}")
            hres = [f"{hTr}_{j}" for j in range(4)]
            sl = slice(tb * 512, (tb + 1) * 512)
            kb, kbr = kblk.next()
            for h in range(8):
                pm, pmr = pmm.next()
                for c in range(8):
                    P.mm(pm[:], W[:, 8 + h, c, :], hT[:, c, :], start=(c == 0), stop=(c == 7),
                         r=hres + [f'W{(8 + h) // 4}'], w=[pmr])
                P.copy('act', kb[:, h, :], pm[:], r=[pmr], w=[kbr])
                P.op('dve', (lambda pm=pm, h=h, tb=tb: (lambda e: e.tensor_reduce(
                    out=kms[:, h, 2 * tb:2 * tb + 2], in_=pm[:].rearrange("p (a b) -> p a b", b=256),
                    axis=AX.X, op=ALU.add)))(), [pmr], ['kms'])
            P.dma('sp', mk_s[:, :, sl], kb[:], r=[kbr], key=kbr)
            qb_, qbr = qblk.next()
            sbt, sbr = selb.next()
            for h in range(8):
                pm, pmr = pmm.next()
                for c in range(8):
                    P.mm(pm[:], W[:, h, c, :], hT[:, c, :], start=(c == 0), stop=(c == 7),
                         r=hres + [f'W{h // 4}'], w=[pmr])
                qf, qfr = q32.next()
                P.copy('act', qf[:], pm[:], r=[pmr], w=[qfr])
                P.ts('dve', qb_[:, h, :], pm[:], scale, None, ALU.mult, r=[pmr], w=[qbr])
                for j in range(4):
                    t = tb * 4 + j
                    qblock = t // 2
                    P.mm(pgt[:, (h * 4 + j) * 16:(h * 4 + j) * 16 + 16], qf[:, j * 128:(j + 1) * 128], kms[:, h, :],
                         r=[qfr, 'kms'], w=['PS:pgt'])
                for j in range(4):
                    t = tb * 4 + j
                    qblock = t // 2
                    g_, gr_ = gm.next()
                    t8_, t8r = t8.next()
                    P.tt('dve', g_[:], pgt[:, (h * 4 + j) * 16:(h * 4 + j) * 16 + 16],
                         cs[:, C_PAST + qblock * 16:C_PAST + qblock * 16 + 16], ALU.add, r=['PS:pgt', 'cs'], w=[gr_])
                    P.op('dve', (lambda t8_=t8_, g_=g_: (lambda e: e.max(out=t8_[:], in_=g_[:])))(), [gr_], [t8r])
                    P.ts('dve', g_[:], g_[:], t8_[:, 2:3], None, ALU.is_ge, r=[gr_, t8r], w=[gr_])
                    P.ts('dve', sbt[:, j, h, :], g_[:], 1.0, -NEG, ALU.subtract, ALU.mult, r=[gr_], w=[sbr])
            P.dma('sp', mq_s[:, :, sl], qb_[:], r=[qbr], key=qbr)
            stb, stbr = selTb.next()
            for j in range(4):
                P.tr(pst[:, j * 128:(j + 1) * 128], sbt[:, j].rearrange("p h n -> p (h n)"), identf, r=[sbr, 'cs'], w=['PS:pst'])
            P.copy('act', stb[:], pst[:], r=['PS:pst'], w=[stbr])
            P.dma('sp', msel_s[:, :, sl].rearrange("h n t -> (h n) t"), stb[:], r=[stbr], key=stbr)
            for j in range(4):
                t = tb * 4 + j
                va, var = vaug.next()
                for half in range(2):
                    pm, pmr = pmm.next()
                    for c in range(8):
                        P.mm(pm[:], hT[:, c, j * 128:(j + 1) * 128], Wv[:, c, half * 512:(half + 1) * 512],
                             start=(c == 0), stop=(c == 7), r=[hres[j], 'Wv'], w=[pmr])
                    P.copy('act' if half else 'dve', va[:, half * 4:half * 4 + 4, 0:128],
                           pm[:].rearrange("p (h d) -> p h d", h=4), r=[pmr], w=[var])
                P.dma('sp', mv_s[t * 128:(t + 1) * 128], va[:], r=[var], key=var)
        P.run()


def phase_moba_attn(nc, cst, sel, mq_s, mk_s, mv_s, msel_s, o2T_s):
    with ExitStack() as es:
        sb = lambda n, s, d: _sb(nc, es, n, s, d)
        cs = sb("cs", [128, NCST], F32)
        selt = sb("selt", [16, 2048], F32)
        selb16 = sb("selb16", [16, 2048], BF16)
        identb = sb("identb", [128, 128], BF16)
        causb = sb("causb", [128, 128], BF16)
        qh = Rot([(sb(f"qh{i}", [128, T], BF16), f"qh{i}") for i in range(2)])
        kh = Rot([(sb(f"kh{i}", [128, T], BF16), f"kh{i}") for i in range(2)])
        vh = Rot([(sb(f"vh{i}", [128, NT, 129], BF16), f"vh{i}") for i in range(2)])
        sh = Rot([(sb(f"sh{i}", [16, T], BF16), f"sh{i}") for i in range(2)])
        oTh = Rot([(sb(f"oTh{i}", [128, T], BF16), f"oTh{i}") for i in range(2)])
        pT = Rot([(sb(f"pT{i}", [128, 128], BF16), f"pT{i}") for i in range(6)])
        rden = Rot([(sb(f"rden{i}", [128, 1], F32), f"rden{i}") for i in range(2)])
        onb = Rot([(sb(f"onb{i}", [128, 128], BF16), f"onb{i}") for i in range(2)])
        banks = [_ps(nc, es, f"bk{i}", [128, 512], F32) for i in range(6)]
        sbanks = Rot([(banks[i], f"PS:sbk{i}") for i in range(4)])
        pacc = Rot([(banks[4 + i], f"PS:pacc{i}") for i in range(2)])
        ptr = Rot([(_ps(nc, es, f"ptr{i}", [128, 1024], BF16), f"PS:ptr{i}") for i in range(2)])

        P = Phase(nc, "ma")
        P.dma('sp', cs[:], cst, w=['cs'], key='cs')
        P.dma('sp', selt[:], sel, w=['selt'], key='selt')
        P.copy('dve', identb[:], cs[:, C_ID:C_ID + 128], r=['cs'], w=['identb'])
        P.copy('dve', causb[:], cs[:, C_MU:C_MU + 128], r=['cs'], w=['causb'])
        P.copy('dve', selb16[:], selt[:], r=['selt'], w=['selb16'])

        def loads(h):
            q, qr = qh.next(); k, kr = kh.next(); v, vr = vh.next(); s, sr = sh.next()
            P.dma('sp', q[:], mq_s[:, h, :], w=[qr], key=qr)
            P.dma('sp', k[:], mk_s[:, h, :], w=[kr], key=kr)
            for i in range(4):
                P.dma('sp', v[:, i * 8:(i + 1) * 8, :],
                      mv_s[i * 1024:(i + 1) * 1024, h, :].rearrange("(t p) d -> p t d", p=128),
                      w=[vr + f"_{i}"], key=vr + f"_{i}")
            P.dma('sp', s[:], msel_s[h], w=[sr], key=sr)
            return (q, qr, k, kr, v, vr, s, sr)

        nxt = loads(0)
        import os
        NHL = int(os.environ.get('MA_HEADS', 8))
        for h in range(NHL):
            q, qr, k, kr, v, vr, s, sr = nxt
            if h + 1 < NHL:
                nxt = loads(h + 1)
            oT, oTr = oTh.next()
            for qt in range(NT):
                qblock = qt // 2
                qs = slice(qt * 128, (qt + 1) * 128)
                pa, par = pacc.next()
                slope_h = 2.0 ** -(h + 1)
                allk = [kt for kt in range(qt + 1) if kt >= qt - 1 or slope_h * ((qt - kt - 1) * 128 + 1) <= 200.0]
                for g0 in range(0, len(allk), 4):
                    kts = allk[g0:g0 + 4]
                    sbk, sbr_ = sbanks.next()
                    for i, kt in enumerate(kts):
                        b = kt // 2
                        ks = slice(kt * 128, (kt + 1) * 128)
                        p1 = sbk[:, i * 128:(i + 1) * 128]
                        nomask = (b == qblock and kt != qt)
                        P.mm(p1, k[:, ks], q[:, qs], start=True, stop=nomask, r=[kr, qr], w=[sbr_])
                        if b < qblock:
                            P.mm(p1, selb16[:, b * 128:(b + 1) * 128], s[:, qs], start=False, stop=True,
                                 r=['selb16', sr], w=[sbr_])
                        elif kt == qt:
                            P.mm(p1, identb[:], causb[:], start=False, stop=True, r=['identb', 'causb'], w=[sbr_])
                    for i, kt in enumerate(kts):
                        p1 = sbk[:, i * 128:(i + 1) * 128]
                        pt, ptr_ = pT.next()
                        P.act(pt[:], p1, AF.Exp, bias=cs[:, C_ALI + h * 32 + (qt - kt):C_ALI + h * 32 + (qt - kt) + 1],
                              r=[sbr_, 'cs'], w=[ptr_])
                        P.mm(pa[:, 0:129], pt[:], v[:, kt, :], start=(kt == allk[0]), stop=(kt == qt),
                             r=[ptr_, vr + f"_{kt // 8}"], w=[par])
                rd, rdr = rden.next()
                ob, obr = onb.next()
                P.op('dve', (lambda rd=rd, pa=pa: (lambda e: e.reciprocal(out=rd[:], in_=pa[:, 128:129])))(), [par], [rdr])
                P.ts('dve', ob[:], pa[:, 0:128], rd[:, 0:1], None, ALU.mult, r=[par, rdr], w=[obr])
                pt2, pt2r = ptr.next()
                P.tr(pt2[:, 0:128], ob[:], identb[:], r=[obr, 'identb'], w=[pt2r])
                P.copy('dve', oT[:, qs], pt2[:, 0:128], r=[pt2r], w=[oTr])
            P.dma('sp', o2T_s[:, h, :], oT[:], r=[oTr], key=oTr)
        P.run()


def phase_outproj(nc, oT_s, wout_d, h_in, h_out):
    with ExitStack() as es:
        sb = lambda n, s, d: _sb(nc, es, n, s, d)
        wout = sb("wout", [128, 8, 1024], BF16)
        ot = Rot([(sb(f"ot{i}", [128, 8, 128], BF16), f"ot{i}") for i in range(3)])
        xt = Rot([(sb(f"xt{i}", [128, D], F32), f"xt{i}") for i in range(3)])
        ho = Rot([(sb(f"ho{i}", [128, D], F32), f"ho{i}") for i in range(2)])
        pb = Rot([(_ps(nc, es, f"pb{i}", [128, 512], F32), f"PS:pb{i}") for i in range(4)])
        P = Phase(nc, "op")
        P.dma('pool', wout[:], wout_d, w=['wout'], key='wout')
        for t in range(NT):
            sl = slice(t * 128, (t + 1) * 128)
            o, orr = ot.next(); xx, xr = xt.next(); hh, hr = ho.next()
            P.dma('sp', o[:], oT_s[:, :, sl], w=[orr], key=orr)
            P.dma('sp', xx[:], h_in[sl, :], w=[xr], key=xr)
            for half in range(2):
                p, pr = pb.next()
                for h in range(8):
                    P.mm(p[:], o[:, h, :], wout[:, h, half * 512:(half + 1) * 512], start=(h == 0), stop=(h == 7),
                         r=[orr, 'wout'], w=[pr])
                P.tt('dve', hh[:, half * 512:(half + 1) * 512], p[:], xx[:, half * 512:(half + 1) * 512], ALU.add,
                     r=[pr, xr], w=[hr])
            P.dma('sp', h_out[sl, :], hh[:], r=[hr], key=hr)
        P.run()


def make_consts():
    c = np.zeros((128, NCST), np.float32)
    j = np.arange(128)[:, None]
    i = np.arange(128)[None, :]
    c[:, C_ID:C_ID + 128] = (i == j)
    c[:, C_MU:C_MU + 128] = np.where(i >= j, 0.0, NEG)
    c[:, C_MSU:C_MSU + 128] = np.where(i > j, 0.0, NEG)
    c[:, C_TRI:C_TRI + 128] = (j <= i)
    c[:, C_ONE:C_ONE + 128] = 1.0
    p = np.arange(128, dtype=np.float64)
    for h in range(8):
        slope = 2.0 ** (-8.0 * (h + 1) / 8)
        for dlt in range(32):
            c[:, C_ALI + h * 32 + dlt] = slope * (p - dlt * 128.0)
    for qb in range(16):
        for n in range(16):
            c[:, C_PAST + qb * 16 + n] = 0.0 if n < qb else -1e30
    s = np.zeros((16, 2048), np.float32)
    for b in range(16):
        s[b, b * 128:(b + 1) * 128] = 1.0
    return c, s


def prep_shared(inp):
    f = lambda a: np.ascontiguousarray(a, dtype=np.float32)
    cst, sel = make_consts()
    w = inp["dn_w_in"][0]
    mw = inp["mb_w_in"][0]
    sh = {
        "gam": f(np.concatenate([inp["mix_norm_g"][0:1], inp["mlp_norm_g"][0:1], inp["mix_norm_g"][1:2],
                                 inp["mlp_norm_g"][1:2], inp["final_norm_g"][None, :]], axis=0)),
        "cst": cst, "sel": sel,
        "dn_win": f(w[:, :4096].reshape(8, 128, 32, 128).transpose(1, 2, 0, 3)),
        "dn_wg": f(w[:, 4096:].reshape(8, 128, 16).transpose(1, 0, 2)),
        "dn_cw": f(inp["dn_conv_w"][0].reshape(4, 24, 128).transpose(2, 1, 0)),
        "dn_vec": f(np.concatenate([inp["dn_a_log"][0], inp["dn_dt_bias"][0]])),
        "dn_ong": f(inp["dn_out_norm_g"][0].reshape(128, 1)),
        "dn_wout": f(inp["dn_w_out"][0].reshape(8, 128, 1024).transpose(1, 0, 2)),
        "mb_wqk": f(mw[:, :2048].reshape(8, 128, 16, 128).transpose(1, 2, 0, 3)),
        "mb_wv": f(mw[:, 2048:].reshape(8, 128, 1024).transpose(1, 0, 2)),
        "mb_wout": f(inp["mb_w_out"][0].reshape(8, 128, 1024).transpose(1, 0, 2)),
        "wup": f(inp["mlp_w_up"].reshape(2, 8, 128, 32, 128).transpose(0, 2, 3, 1, 4)),
        "wdn": f(inp["mlp_w_down"].reshape(2, 32, 128, 1024).transpose(0, 2, 1, 3)),
    }
    return sh


_NC_CACHE = {}


def kernel(**inputs):
    inp = {k: np.asarray(v) for k, v in inputs.items()}
    sh = prep_shared(inp)
    if "nc" not in _NC_CACHE:
        _NC_CACHE["nc"] = build()
    nc = _NC_CACHE["nc"]
    x = np.ascontiguousarray(inp["x"], dtype=np.float32)
    in_maps = [dict(sh, x=x[b]) for b in range(8)]
    res = run_bass_kernel_spmd(nc, in_maps, core_ids=list(range(8)))
    return np.stack([np.asarray(r["out"]) for r in res.results], axis=0).astype(np.float32)
```

```python
import numpy as np
from contextlib import ExitStack
import concourse.bass as bass
import concourse.mybir as mybir
from concourse.bass_utils import run_bass_kernel_spmd

F32 = mybir.dt.float32
BF16 = mybir.dt.bfloat16
AF = mybir.ActivationFunctionType
ALU = mybir.AluOpType
AX = mybir.AxisListType

T = 4096
D = 1024
NT = 32
H = 8
NEG = -30000.0
EPS = 1e-6

C_ID, C_MU, C_MSU, C_TRI, C_ONE, C_ALI, C_PAST = 0, 128, 256, 384, 512, 640, 896
NCST = 896 + 256


class Phase:
    def __init__(self, nc, name):
        self.nc = nc
        self.name = name
        self.ops = []

    def op(self, eng, fn, r=(), w=(), dma=False, key=None):
        self.ops.append(dict(eng=eng, fn=fn, reads=tuple(r), writes=tuple(w), dma=dma, key=key, signal=False))

    def mm(self, out, lhsT, rhs, start=True, stop=True, r=(), w=()):
        self.op('pe', lambda e: e.matmul(out, lhsT=lhsT, rhs=rhs, start=start, stop=stop), r, w)

    def tr(self, out, in_, ident, r=(), w=()):
        self.op('pe', lambda e: e.transpose(out=out, in_=in_, identity=ident), r, w)

    def act(self, out, in_, func, r=(), w=(), bias=None, scale=None, accum=None):
        kw = {}
        if bias is not None:
            kw['bias'] = bias
        if scale is not None:
            kw['scale'] = scale
        if accum is not None:
            kw['accum_out'] = accum
        self.op('act', lambda e: e.activation(out=out, in_=in_, func=func, **kw), r, w)

    def ts(self, eng, out, in0, s1, s2, op0, op1=None, r=(), w=()):
        if op1 is None:
            self.op(eng, lambda e: e.tensor_scalar(out=out, in0=in0, scalar1=s1, scalar2=None, op0=op0), r, w)
        else:
            self.op(eng, lambda e: e.tensor_scalar(out=out, in0=in0, scalar1=s1, scalar2=s2, op0=op0, op1=op1), r, w)

    def tt(self, eng, out, in0, in1, op, r=(), w=()):
        self.op(eng, lambda e: e.tensor_tensor(out=out, in0=in0, in1=in1, op=op), r, w)

    def stt(self, eng, out, in0, scalar, in1, op0, op1, r=(), w=()):
        self.op(eng, lambda e: e.scalar_tensor_tensor(out=out, in0=in0, scalar=scalar, in1=in1, op0=op0, op1=op1), r, w)

    def copy(self, eng, out, in_, r=(), w=()):
        if eng == 'act':
            self.op(eng, lambda e: e.copy(out=out, in_=in_), r, w)
        else:
            self.op(eng, lambda e: e.tensor_copy(out=out, in_=in_), r, w)

    def memset(self, eng, ap, val, w=()):
        self.op(eng, lambda e: e.memset(ap, val), (), w)

    def dma(self, eng, out, in_, r=(), w=(), key=None):
        self.op(eng, lambda e: e.dma_start(out=out, in_=in_), r, w, dma=True, key=key)

    def run(self):
        nc = self.nc
        ops = self.ops
        last_w = {}
        readers = {}
        deps = []
        for i, o in enumerate(ops):
            d = set()
            excl = [r for r in o['reads'] if r.startswith("PS:")]
            if excl:
                o['writes'] = tuple(o['writes']) + tuple(x for x in excl if x not in o['writes'])
            for r in o['reads']:
                if r in last_w:
                    d.add(last_w[r])
            for w in o['writes']:
                if w in last_w:
                    d.add(last_w[w])
                d.update(readers.get(w, ()))
            d.discard(i)
            for r in o['reads']:
                readers.setdefault(r, []).append(i)
            for w in o['writes']:
                last_w[w] = i
                readers[w] = []
            deps.append(d)
        eidx = []
        ecount = {}
        for o in ops:
            ecount[o['eng']] = ecount.get(o['eng'], 0) + 1
            eidx.append(ecount[o['eng']])

        def same_ok(i, d):
            o, od = ops[i], ops[d]
            if od['eng'] != o['eng'] or o['dma']:
                return False
            if o['eng'] == 'pe':
                return True
            return eidx[i] - eidx[d] > 6

        for i, o in enumerate(ops):
            for d in deps[i]:
                od = ops[d]
                if od['dma']:
                    continue
                if same_ok(i, d):
                    continue
                od['signal'] = True
        lastop = {}
        for o in ops:
            if not o['dma']:
                lastop[o['eng']] = o
        for o in lastop.values():
            o['signal'] = True
        cnt = {}
        dcnt = {}
        for o in ops:
            if o['dma']:
                dcnt[o['key']] = dcnt.get(o['key'], 0) + 16
                o['dval'] = dcnt[o['key']]
            elif o['signal']:
                cnt[o['eng']] = cnt.get(o['eng'], 0) + 1
                o['cval'] = cnt[o['eng']]
        engs = ('pe', 'act', 'dve', 'pool', 'sp')
        with ExitStack() as es:
            esem = {e: nc.alloc_semaphore(name=f"{self.name}_s_{e}") for e in engs if cnt.get(e)}
            dsem = {k: nc.alloc_semaphore(name=f"{self.name}_d_{j}") for j, k in enumerate(dcnt)}
            seen = {e: {} for e in engs}
            for i, o in enumerate(ops):
                w = {}
                for d in deps[i]:
                    od = ops[d]
                    if od['dma']:
                        s, v = ('d', od['key']), od['dval']
                    elif same_ok(i, d):
                        continue
                    else:
                        s, v = ('e', od['eng']), od['cval']
                    if seen[o['eng']].get(s, 0) >= v:
                        continue
                    w[s] = max(w.get(s, 0), v)
                for s, v in w.items():
                    seen[o['eng']][s] = v
                o['waits'] = [((dsem[s[1]] if s[0] == 'd' else esem[s[1]]), v) for s, v in w.items()]
            fence = [(dsem[k], v) for k, v in dcnt.items()] + [(esem[e], cnt[e]) for e in esem]
            block = es.enter_context(nc.Block())

            def make(ename):
                def body(eng):
                    for o in ops:
                        if o['eng'] != ename:
                            continue
                        for s, v in o['waits']:
                            eng.wait_ge(s, v)
                        ins = o['fn'](eng)
                        if o['dma']:
                            ins.then_inc(dsem[o['key']], 16)
                        elif o['signal']:
                            ins.then_inc(esem[ename], 1)
                    if ename == 'sp':
                        for s, v in fence:
                            eng.wait_ge(s, v)
                return body

            block.tensor(make('pe'))
            block.scalar(make('act'))
            block.vector(make('dve'))
            block.gpsimd(make('pool'))
            block.sync(make('sp'))
        nc.all_engine_barrier()
        nc.clear_and_free_semaphores(list(esem.values()) + list(dsem.values()))
        nc.all_engine_barrier()
        return len(ops)


class Rot:
    def __init__(self, items):
        self.items = items
        self.i = 0

    def next(self):
        r = self.items[self.i % len(self.items)]
        self.i += 1
        return r


_UID = [0]


def _sb(nc, es, name, shape, dt):
    _UID[0] += 1
    return es.enter_context(nc.sbuf_tensor(f"{name}_u{_UID[0]}", list(shape), dt))


def _ps(nc, es, name, shape, dt):
    _UID[0] += 1
    return es.enter_context(nc.psum_tensor(f"{name}_u{_UID[0]}", list(shape), dt))


def emit_norm_T(P, src, gamt, xin, hn, stat, junk, ptr, hnT_dst, identb, tag, evac_eng, hnT_res, keep=None):
    xt, xr = xin
    hnt, hr = hn
    stt_, sr = stat
    pt, pr = ptr
    P.dma('sp', xt[:], src, w=[xr], key=xr)
    P.act(junk[:], xt[:], AF.Square, accum=stt_[:, 0:1], r=[xr], w=[sr])
    P.ts('dve', stt_[:, 1:2], stt_[:, 0:1], 1.0 / D, EPS, ALU.mult, ALU.add, r=[sr], w=[sr])
    P.act(stt_[:, 2:3], stt_[:, 1:2], AF.Sqrt, r=[sr], w=[sr])
    P.op('dve', lambda e: e.reciprocal(out=stt_[:, 3:4], in_=stt_[:, 2:3]), [sr], [sr])
    P.stt('dve', hnt[:], xt[:], stt_[:, 3:4], gamt[:], ALU.mult, ALU.mult, r=[xr, sr, 'gam'], w=[hr])
    for c in range(8):
        P.tr(pt[:, c * 128:(c + 1) * 128], hnt[:, c * 128:(c + 1) * 128], identb[:], r=[hr, 'identb'], w=[pr])
    P.copy(evac_eng, hnT_dst, pt[:].rearrange("p (c t) -> p c t", c=8), r=[pr], w=[hnT_res])


def build(debug=False, upto=99):
    nc = bass.Bass("TRN2", target_bir_lowering=False)

    def din(name, shape, dt=F32):
        return nc.dram_tensor(name, list(shape), dt, kind="ExternalInput").ap()

    import os
    keep = os.environ.get("DBG_OUT", "").split(",")

    def dscr(name, shape, dt):
        ext = debug and (keep == [""] or name in keep)
        return nc.dram_tensor(name, list(shape), dt, kind=("ExternalOutput" if ext else "Internal")).ap()

    x = din("x", [T, D])
    gam = din("gam", [5, D])
    cst = din("cst", [128, NCST])
    sel = din("sel", [16, 2048])
    dn_win = din("dn_win", [128, 32, 8, 128])
    dn_wg = din("dn_wg", [128, 8, 16])
    dn_cw = din("dn_cw", [128, 24, 4])
    dn_vec = din("dn_vec", [16])
    dn_ong = din("dn_ong", [128, 1])
    dn_wout = din("dn_wout", [128, 8, 1024])
    mb_wqk = din("mb_wqk", [128, 16, 8, 128])
    mb_wv = din("mb_wv", [128, 8, 1024])
    mb_wout = din("mb_wout", [128, 8, 1024])
    wup = din("wup", [2, 128, 32, 8, 128])
    wdn = din("wdn", [2, 128, 32, 1024])
    out = nc.dram_tensor("out", [T, D], F32, kind="ExternalOutput").ap()

    qT_s = dscr("qT_s", [128, 8, T], F32)
    kT_s = dscr("kT_s", [128, 8, T], F32)
    vT_s = dscr("vT_s", [128, 8, T], F32)
    szT_s = dscr("szT_s", [128, 8, T], BF16)
    graw_s = dscr("graw_s", [128, NT, 16], F32)
    ss_s = dscr("ss_s", [128, NT, 16], F32)
    oT_s = dscr("oT_s", [128, 8, T], BF16)
    h1_s = dscr("h1_s", [T, D], F32)
    h2_s = dscr("h2_s", [T, D], F32)
    h3_s = dscr("h3_s", [T, D], F32)
    mq_s = dscr("mq_s", [128, 8, T], BF16)
    mk_s = dscr("mk_s", [128, 8, T], BF16)
    mv_s = dscr("mv_s", [T, 8, 129], BF16)
    msel_s = dscr("msel_s", [8, 16, T], BF16)
    o2T_s = dscr("o2T_s", [128, 8, T], BF16)

    only = os.environ.get('ONLY')
    run_ = lambda n: upto >= n and (only is None or int(only) == n)
    if run_(1) and not os.environ.get('SKIP1'):
        phase_gdn_proj(nc, x, gam, cst, dn_win, dn_wg, dn_cw, qT_s, kT_s, vT_s, szT_s, graw_s, ss_s)
    if run_(2):
        phase_gdn_core(nc, x, cst, sel, dn_vec, dn_ong, dn_wout, qT_s, kT_s, vT_s, szT_s, graw_s, ss_s, h1_s)
    if run_(3):
        phase_mlp(nc, "m0", h1_s, h2_s, gam[1], None, cst, wup[0], wdn[0])
    if run_(4):
        phase_moba_proj(nc, h2_s, gam, cst, mb_wqk, mb_wv, mq_s, mk_s, mv_s, msel_s)
    if run_(5):
        phase_moba_attn(nc, cst, sel, mq_s, mk_s, mv_s, msel_s, o2T_s)
    if run_(6):
        phase_outproj(nc, o2T_s, mb_wout, h2_s, h3_s)
    if run_(7):
        phase_mlp(nc, "m1", h3_s, out, gam[3], gam[4], cst, wup[1], wdn[1])
    return nc


def phase_gdn_proj(nc, x, gam, cst, dn_win, dn_wg, dn_cw, qT_s, kT_s, vT_s, szT_s, graw_s, ss_s):
    with ExitStack() as es:
        sb = lambda n, s, d: _sb(nc, es, n, s, d)
        W = sb("W", [128, 32, 8, 128], BF16)
        Wg = sb("Wg", [128, 8, 16], BF16)
        gamt = sb("gamt", [128, D], F32)
        cw = sb("cw", [128, 24, 4], F32)
        cs = sb("cs", [128, 640], F32)
        identb = sb("identb", [128, 128], BF16)
        halo = sb("halo", [128, 24, 3], F32)
        graw = sb("graw", [128, NT, 16], F32)
        ssraw = sb("ssraw", [128, NT, 16], F32)
        xin = Rot([(sb(f"xin{i}", [128, D], F32), f"xin{i}") for i in range(2)])
        hn = Rot([(sb(f"hn{i}", [128, D], BF16), f"hn{i}") for i in range(2)])
        stat = Rot([(sb(f"stat{i}", [128, 4], F32), f"stat{i}") for i in range(2)])
        junk = sb("junk", [128, D], BF16)
        hnT = Rot([(sb(f"hnT{i}", [128, 8, 512], BF16), f"hnT{i}") for i in range(2)])
        praw = Rot([(sb(f"praw{i}", [128, 515], F32), f"praw{i}") for i in range(3)])
        yv = Rot([(sb(f"yv{i}", [128, 512], F32), f"yv{i}") for i in range(3)])
        sy = Rot([(sb(f"sy{i}", [128, 512], F32), f"sy{i}") for i in range(3)])
        sq = Rot([(sb(f"sq{i}", [128, 512], F32), f"sq{i}") for i in range(2)])
        szb = Rot([(sb(f"szb{i}", [128, 512], BF16), f"szb{i}") for i in range(2)])
        ptr = Rot([(_ps(nc, es, f"ptr{i}", [128, 1024], BF16), f"PS:ptr{i}") for i in range(2)])
        pmm = Rot([(_ps(nc, es, f"pmm{i}", [128, 512], F32), f"PS:pmm{i}") for i in range(4)])
        pss = _ps(nc, es, "pss", [128, 512], F32)[:, 0:64].rearrange("p (j c) -> p j c", j=4)
        pg = _ps(nc, es, "pg", [128, 512], F32)[:, 0:64].rearrange("p (j c) -> p j c", j=4)

        P = Phase(nc, "gp")
        P.dma('sp', cs[:], cst[:, 0:640], w=['cs'], key='cs')
        P.dma('sp', gamt[:], gam[0].partition_broadcast(128), w=['gam'], key='gam')
        P.dma('sp', cw[:], dn_cw, w=['cw'], key='cw')
        for i in range(8):
            P.dma('pool', W[:, 4 * i:4 * i + 4], dn_win[:, 4 * i:4 * i + 4], w=[f'W{i}'], key=f'W{i}')
        P.dma('pool', Wg[:], dn_wg, w=['Wg'], key='Wg')
        P.copy('dve', identb[:], cs[:, C_ID:C_ID + 128], r=['cs'], w=['identb'])
        P.memset('pool', halo[:], 0.0, w=['halo%d' % c_ for c_ in range(24)])
        ones_col = cs[:, C_ONE:C_ONE + 1]
        for tb in range(8):
            hT, hTr = hnT.next()
            for j in range(4):
                t = tb * 4 + j
                emit_norm_T(P, x[t * 128:(t + 1) * 128, :], gamt, xin.next(), hn.next(), stat.next(), junk,
                            ptr.next(), hT[:, :, j * 128:(j + 1) * 128], identb, "gp", 'act' if j % 2 else 'dve',
                            f"{hTr}_{j}")
            hres = [f"{hTr}_{j}" for j in range(4)]
            for j in range(4):
                for c in range(8):
                    P.mm(pg[:, j, :], hT[:, c, j * 128:(j + 1) * 128], Wg[:, c, :], start=(c == 0), stop=(c == 7),
                         r=[hres[j], 'Wg'], w=['PS:pg'])
            P.copy('act', graw[:, tb * 4:tb * 4 + 4, :], pg, r=['PS:pg'], w=['graw'])
            for cc in range(32):
                pm, pmr = pmm.next()
                for c in range(8):
                    P.mm(pm[:], W[:, cc, c, :], hT[:, c, :], start=(c == 0), stop=(c == 7),
                         r=hres + [f'W{cc // 4}'], w=[pmr])
                h = cc % 8
                sl = slice(tb * 512, (tb + 1) * 512)
                if cc < 24:
                    pr_, prr = praw.next()
                    y, yr = yv.next()
                    s, sr = sy.next()
                    ce = 'dve'
                    P.copy('act', pr_[:, 3:515], pm[:], r=[pmr], w=[prr])
                    P.copy(ce, pr_[:, 0:3], halo[:, cc, :], r=['halo%d' % cc], w=[prr])
                    P.copy(ce, halo[:, cc, :], pr_[:, 512:515], r=[prr], w=['halo%d' % cc])
                    P.ts(ce, y[:], pr_[:, 0:512], cw[:, cc, 0:1], None, ALU.mult, r=[prr, 'cw'], w=[yr])
                    for k in range(1, 4):
                        P.stt(ce, y[:], pr_[:, k:k + 512], cw[:, cc, k:k + 1], y[:], ALU.mult, ALU.add,
                              r=[prr, 'cw', yr], w=[yr])
                    P.act(s[:], y[:], AF.Silu, r=[yr], w=[sr])
                    dst = (qT_s, kT_s, vT_s)[cc // 8]
                    P.dma('sp', dst[:, h, sl], s[:], r=[sr], w=[], key=sr)
                    if cc < 16:
                        q2, q2r = sq.next()
                        P.tt('pool', q2[:], s[:], s[:], ALU.mult, r=[sr], w=[q2r])
                        for j in range(4):
                            P.mm(pss[:, j, cc:cc + 1], q2[:, j * 128:(j + 1) * 128], ones_col, r=[q2r, 'cs'], w=['PS:pss'])
                        if cc == 15:
                            P.copy('dve', ssraw[:, tb * 4:tb * 4 + 4, :], pss, r=['PS:pss'], w=['ssraw'])
                else:
                    zb, zr = szb.next()
                    P.act(zb[:], pm[:], AF.Silu, r=[pmr], w=[zr])
                    P.dma('sp', szT_s[:, h, sl], zb[:], r=[zr], w=[], key=zr)
        P.dma('sp', graw_s, graw[:], r=['graw'], key='graw_o')
        P.dma('sp', ss_s, ssraw[:], r=['ssraw'], key='ss_o')
        P.run()


def phase_gdn_core(nc, x, cst, sel, dn_vec, dn_ong, dn_wout, qT_s, kT_s, vT_s, szT_s, graw_s, ss_s, h1_s):
    with ExitStack() as es:
        sb = lambda n, s, d: _sb(nc, es, n, s, d)
        cs = sb("cs", [128, 640], F32)
        identb = sb("identb", [128, 128], BF16)
        wout = sb("wout", [128, 8, 1024], BF16)
        ong = sb("ong", [128, 1], F32)
        vecb = sb("vecb", [128, 16], F32)
        graw = sb("graw", [128, NT, 16], F32)
        ssr = sb("ssr", [128, NT, 16], F32)
        G = {n: sb("g_" + n, [128, NT, 8], F32) for n in
             ("l1", "g", "gc", "gl", "lrq", "lrk", "bj", "ckbg", "ckdec", "cvb", "co1", "egt", "tmp", "tmp2")}
        rsrc = sb("rsrc", [128, NT, 16], F32)
        S = sb("S", [128, 8, 128], F32)
        qt_ = Rot([(sb(f"q{i}", [128, 8, 128], F32), f"q{i}") for i in range(2)])
        kt_ = Rot([(sb(f"k{i}", [128, 8, 128], F32), f"k{i}") for i in range(2)])
        vt_ = Rot([(sb(f"v{i}", [128, 8, 128], F32), f"v{i}") for i in range(2)])
        zt_ = Rot([(sb(f"z{i}", [128, 8, 128], BF16), f"z{i}") for i in range(2)])
        xt_ = Rot([(sb(f"x{i}", [128, D], F32), f"x{i}") for i in range(2)])
        ho_ = Rot([(sb(f"ho{i}", [128, D], F32), f"ho{i}") for i in range(2)])
        oTg = Rot([(sb(f"oTg{i}", [128, 8, 128], BF16), f"oTg{i}") for i in range(2)])
        names = ("kbg", "kdec", "vb", "DmU", "DmB", "M", "attnT", "A", "PA0", "PA1", "PM0", "PM1", "N0", "N1",
                 "nwT", "vnew", "O2", "o", "on", "dg1", "dg2", "usb", "ktm")
        Wk = {n: [sb(f"w_{n}{h}", [128, 128], F32) for h in range(8)] for n in names}
        ojunk = sb("ojunk", [128, 128], F32)
        ost = sb("ost", [128, 8, 4], F32)
        banks = [_ps(nc, es, f"bk{i}", [128, 512], F32) for i in range(8)]
        pbanks = Rot([(banks[i], f"PS:bk{i}") for i in range(6)])
        pbig = Rot([(banks[6 + i], f"PS:pb{i}") for i in range(2)])

        P = Phase(nc, "gc")
        P.dma('sp', cs[:], cst[:, 0:640], w=['cs'], key='cs')
        P.dma('sp', ong[:], dn_ong, w=['ong'], key='ong')
        P.dma('sp', vecb[:], dn_vec.partition_broadcast(128), w=['vecb'], key='vecb')
        P.dma('sp', graw[:], graw_s, w=['graw'], key='graw')
        P.dma('sp', ssr[:], ss_s, w=['ssr'], key='ssr')
        P.dma('pool', wout[:], dn_wout, w=['wout'], key='wout')
        P.copy('dve', identb[:], cs[:, C_ID:C_ID + 128], r=['cs'], w=['identb'])
        P.memset('pool', S[:], 0.0, w=[f'S{h}' for h in range(8)])
        identf = cs[:, C_ID:C_ID + 128]
        maskU = cs[:, C_MU:C_MU + 128]
        maskSU = cs[:, C_MSU:C_MSU + 128]
        tri = cs[:, C_TRI:C_TRI + 128]
        ones = cs[:, C_ONE:C_ONE + 128]

        b_raw = graw[:, :, 0:8]
        a_raw = graw[:, :, 8:16]
        gr = ['graw', 'vecb', 'ssr', 'cs']
        P.act(G["tmp"][:], b_raw, AF.Exp, scale=-1.0, r=gr, w=['g_tmp'])
        P.act(G["l1"][:], G["tmp"][:], AF.Ln, bias=1.0, r=['g_tmp'], w=['g_l1'])
        P.act(G["cvb"][:], G["l1"][:], AF.Exp, scale=-1.0, r=['g_l1'], w=['g_cvb'])
        for t in range(NT):
            P.tt('dve', G["tmp2"][:, t, :], a_raw[:, t, :], vecb[:, 8:16], ALU.add, r=gr, w=['g_tmp2'])
        P.act(G["tmp"][:], G["tmp2"][:], AF.Exp, r=['g_tmp2'], w=['g_tmp'])
        P.act(G["tmp2"][:], G["tmp"][:], AF.Ln, bias=1.0, r=['g_tmp'], w=['g_tmp2'])
        P.act(vecb[:, 0:8], vecb[:, 0:8], AF.Exp, r=['vecb'], w=['vecb2'])
        for t in range(NT):
            P.stt('dve', G["g"][:, t, :], G["tmp2"][:, t, :], -1.0, vecb[:, 0:8], ALU.mult, ALU.mult,
                  r=['g_tmp2', 'vecb2'], w=['g_g'])
        g2 = G["g"][:].rearrange("p t h -> p (t h)")
        pb, pbr = pbig.next()
        P.mm(pb[:, 0:256], tri, g2, r=['g_g', 'cs'], w=[pbr])
        P.copy('dve', G["gc"][:].rearrange("p t h -> p (t h)"), pb[:, 0:256], r=[pbr], w=['g_gc'])
        pb2, pb2r = pbig.next()
        P.mm(pb2[:, 0:256], ones, g2, r=['g_g', 'cs'], w=[pb2r])
        P.copy('dve', G["gl"][:].rearrange("p t h -> p (t h)"), pb2[:, 0:256], r=[pb2r], w=['g_gl'])
        P.ts('dve', G["tmp"][:], ssr[:, :, 0:8], EPS, None, ALU.add, r=gr, w=['g_tmp'])
        P.act(G["tmp"][:], G["tmp"][:], AF.Ln, r=['g_tmp'], w=['g_tmp'])
        P.ts('dve', G["lrq"][:], G["tmp"][:], -0.5, float(np.log(128.0 ** -0.5)), ALU.mult, ALU.add, r=['g_tmp'], w=['g_lrq'])
        P.ts('dve', G["tmp"][:], ssr[:, :, 8:16], EPS, None, ALU.add, r=gr + ['g_lrq'], w=['g_tmp'])
        P.act(G["tmp"][:], G["tmp"][:], AF.Ln, r=['g_tmp'], w=['g_tmp'])
        P.ts('dve', G["lrk"][:], G["tmp"][:], -0.5, None, ALU.mult, r=['g_tmp'], w=['g_lrk'])
        P.tt('dve', rsrc[:, :, 0:8], G["gc"][:], G["lrq"][:], ALU.add, r=['g_gc', 'g_lrq'], w=['rsrc'])
        P.tt('dve', G["tmp"][:], G["gc"][:], G["l1"][:], ALU.subtract, r=['g_gc', 'g_l1', 'g_lrk'], w=['g_tmp'])
        P.tt('dve', rsrc[:, :, 8:16], G["tmp"][:], G["lrk"][:], ALU.add, r=['g_tmp', 'g_lrk'], w=['rsrc'])
        P.tt('dve', G["bj"][:], G["lrk"][:], G["gc"][:], ALU.subtract, r=['g_gc', 'g_lrk'], w=['g_bj'])
        P.act(G["ckbg"][:], rsrc[:, :, 8:16], AF.Exp, r=['rsrc'], w=['g_ckbg'])
        P.act(G["co1"][:], rsrc[:, :, 0:8], AF.Exp, r=['rsrc'], w=['g_co1'])
        P.tt('dve', G["tmp2"][:], G["gl"][:], G["bj"][:], ALU.add, r=['g_gl', 'g_bj', 'g_g'], w=['g_tmp2'])
        P.act(G["ckdec"][:], G["tmp2"][:], AF.Exp, r=['g_tmp2'], w=['g_ckdec'])
        P.act(G["egt"][:], G["gl"][:], AF.Exp, r=['g_gl'], w=['g_egt'])
        gall = ['g_ckbg', 'g_co1', 'g_ckdec', 'g_egt', 'g_cvb', 'g_bj', 'rsrc']

        def loads(t):
            sl = slice(t * 128, (t + 1) * 128)
            q, qr = qt_.next(); k, kr = kt_.next(); v, vr = vt_.next(); z, zr = zt_.next(); xx, xr = xt_.next()
            P.dma('sp', q[:], qT_s[:, :, sl], w=[qr], key=qr)
            P.dma('sp', k[:], kT_s[:, :, sl], w=[kr], key=kr)
            P.dma('sp', v[:], vT_s[:, :, sl], w=[vr], key=vr)
            P.dma('sp', z[:], szT_s[:, :, sl], w=[zr], key=zr)
            P.dma('sp', xx[:], x[sl, :], w=[xr], key=xr)
            return (q, qr, k, kr, v, vr, z, zr, xx, xr)

        nxt = loads(0)
        import os
        HS = list(range(int(os.environ.get('GC_HEADS', 8))))
        NTL = int(os.environ.get('GC_TILES', NT))
        STG = int(os.environ.get('GC_STAGE', 99))
        groups = [HS[i:i + 4] for i in range(0, len(HS), 4)]
        rs = lambda n, h: f"w_{n}{h}"

        def stage(mmfn, consfns):
            for grp in groups:
                bk, bkr = pbanks.next()
                for i, h in enumerate(grp):
                    mmfn(h, bk[:, i * 128:(i + 1) * 128], bkr)
                for cf in consfns:
                    for i, h in enumerate(grp):
                        cf(h, bk[:, i * 128:(i + 1) * 128], bkr)

        for t in range(NTL):
            q, qr, k, kr, v, vr, z, zr, xx, xr = nxt
            if t + 1 < NTL:
                nxt = loads(t + 1)
            if STG < 1:
                continue
            stage(lambda h, pq, br: P.tr(pq, k[:, h, :], identf, r=[kr, 'cs'], w=[br]),
                  [lambda h, pq, br: P.copy('act', Wk["ktm"][h][:], pq, r=[br], w=[rs("ktm", h)])])
            for h in HS:
                P.ts('pool', Wk["kbg"][h][:], Wk["ktm"][h][:], G["ckbg"][:, t, h:h + 1], None, ALU.mult,
                     r=[rs("ktm", h)] + gall, w=[rs("kbg", h)])
                P.ts('pool', Wk["kdec"][h][:], Wk["ktm"][h][:], G["ckdec"][:, t, h:h + 1], None, ALU.mult,
                     r=[rs("ktm", h)] + gall, w=[rs("kdec", h)])
            stage(lambda h, pq, br: P.tr(pq, v[:, h, :], identf, r=[vr, 'cs'], w=[br]),
                  [lambda h, pq, br: P.act(Wk["vb"][h][:], pq, AF.Copy, scale=G["cvb"][:, t, h:h + 1], r=[br] + gall,
                                           w=[rs("vb", h)])])
            if STG < 2:
                continue
            for dgn, col0, msk, dst in (("dg1", 0, maskU, "DmU"), ("dg2", 8, maskSU, "DmB")):
                for h in HS:
                    P.ts('pool', Wk[dgn][h][:], identf, rsrc[:, t, col0 + h:col0 + h + 1], None, ALU.mult,
                         r=['cs', 'rsrc'], w=[rs(dgn, h)])
                stage(lambda h, pq, br, dgn=dgn: P.mm(pq, ones, Wk[dgn][h][:], r=['cs', rs(dgn, h)], w=[br]),
                      [lambda h, pq, br, dst=dst, msk=msk: P.stt('dve', Wk[dst][h][:], pq, G["bj"][:, t, h:h + 1], msk,
                                                               ALU.add, ALU.add, r=[br, 'cs'] + gall, w=[rs(dst, h)])])
                for h in HS:
                    P.act(Wk[dst][h][:], Wk[dst][h][:], AF.Exp, r=[rs(dst, h)], w=[rs(dst, h)])
            stage(lambda h, pq, br: P.mm(pq, k[:, h, :], k[:, h, :], r=[kr], w=[br]),
                  [lambda h, pq, br: P.tt('dve', Wk["M"][h][:], pq, Wk["DmB"][h][:], ALU.mult, r=[br, rs("DmB", h)],
                                          w=[rs("M", h)])])
            stage(lambda h, pq, br: P.mm(pq, k[:, h, :], q[:, h, :], r=[kr, qr], w=[br]),
                  [lambda h, pq, br: P.tt('dve', Wk["attnT"][h][:], pq, Wk["DmU"][h][:], ALU.mult, r=[br, rs("DmU", h)],
                                          w=[rs("attnT", h)])])
            if STG < 3:
                continue
            stage(lambda h, pq, br: P.tr(pq, Wk["M"][h][:], identf, r=[rs("M", h), 'cs'], w=[br]),
                  [lambda h, pq, br: P.copy('act', Wk["A"][h][:], pq, r=[br], w=[rs("A", h)])])
            for h in HS:
                P.tt('pool', Wk["N0"][h][:], identf, Wk["M"][h][:], ALU.subtract, r=['cs', rs("M", h)], w=[rs("N0", h)])
            if STG < 4:
                continue
            PAc = {h: (Wk["A"][h], rs("A", h)) for h in HS}
            PMc = {h: (Wk["M"][h], rs("M", h)) for h in HS}
            Nc = {h: (Wk["N0"][h], rs("N0", h)) for h in HS}
            for lvl in range(6):
                last = (lvl == 5)
                npa = {h: (Wk["PA%d" % (lvl % 2)][h], rs("PA%d" % (lvl % 2), h)) for h in HS}
                npm = {h: (Wk["PM%d" % (lvl % 2)][h], rs("PM%d" % (lvl % 2), h)) for h in HS}
                nnn = {h: (Wk["N%d" % ((lvl + 1) % 2)][h], rs("N%d" % ((lvl + 1) % 2), h)) for h in HS}
                stage(lambda h, pq, br: P.mm(pq, PMc[h][0][:], PAc[h][0][:], r=[PMc[h][1], PAc[h][1]], w=[br]),
                      [lambda h, pq, br: P.copy('act', npa[h][0][:], pq, r=[br], w=[npa[h][1]])])
                if not last:
                    stage(lambda h, pq, br: P.mm(pq, PAc[h][0][:], PMc[h][0][:], r=[PMc[h][1], PAc[h][1]], w=[br]),
                          [lambda h, pq, br: P.copy('dve', npm[h][0][:], pq, r=[br], w=[npm[h][1]])])
                stage(lambda h, pq, br: P.mm(pq, npa[h][0][:], Nc[h][0][:], r=[npa[h][1], Nc[h][1]], w=[br]),
                      [lambda h, pq, br: P.tt('dve', nnn[h][0][:], pq, Nc[h][0][:], ALU.add, r=[br, Nc[h][1]],
                                              w=[nnn[h][1]])])
                PAc = npa
                if not last:
                    PMc = npm
                Nc = nnn
            if STG < 5:
                continue
            stage(lambda h, pq, br: P.mm(pq, Wk["kbg"][h][:], Nc[h][0][:], r=[rs("kbg", h), Nc[h][1]], w=[br]),
                  [lambda h, pq, br: P.ts('dve', Wk["nwT"][h][:], pq, -1.0, None, ALU.mult, r=[br], w=[rs("nwT", h)])])
            if STG < 6:
                continue
            stage(lambda h, pq, br: P.mm(pq, Nc[h][0][:], Wk["vb"][h][:], r=[Nc[h][1], rs("vb", h)], w=[br]),
                  [lambda h, pq, br: P.copy('act', Wk["usb"][h][:], pq, r=[br], w=[rs("usb", h)])])
            stage(lambda h, pq, br: P.mm(pq, Wk["nwT"][h][:], S[:, h, :], r=[rs("nwT", h), f'S{h}'], w=[br]),
                  [lambda h, pq, br: P.tt('dve', Wk["vnew"][h][:], pq, Wk["usb"][h][:], ALU.add, r=[br, rs("usb", h)],
                                          w=[rs("vnew", h)])])
            stage(lambda h, pq, br: P.mm(pq, Wk["attnT"][h][:], Wk["vnew"][h][:], r=[rs("attnT", h), rs("vnew", h)], w=[br]),
                  [lambda h, pq, br: P.copy('act', Wk["O2"][h][:], pq, r=[br], w=[rs("O2", h)])])
            stage(lambda h, pq, br: P.mm(pq, q[:, h, :], S[:, h, :], r=[qr, f'S{h}'], w=[br]),
                  [lambda h, pq, br: P.stt('dve', Wk["o"][h][:], pq, G["co1"][:, t, h:h + 1], Wk["O2"][h][:], ALU.mult,
                                           ALU.add, r=[br, rs("O2", h)] + gall, w=[rs("o", h)])])
            stage(lambda h, pq, br: P.mm(pq, Wk["kdec"][h][:], Wk["vnew"][h][:], r=[rs("kdec", h), rs("vnew", h)], w=[br]),
                  [lambda h, pq, br: P.stt('dve', S[:, h, :], S[:, h, :], G["egt"][:, t, h:h + 1], pq, ALU.mult, ALU.add,
                                           r=[br, f'S{h}'] + gall, w=[f'S{h}'])])
            for h in HS:
                P.act(ojunk[:], Wk["o"][h][:], AF.Square, accum=ost[:, h, 0:1], r=[rs("o", h)], w=['ost'])
            if STG < 7:
                continue
            P.ts('dve', ost[:, :, 1:2], ost[:, :, 0:1], 1.0 / 128, EPS, ALU.mult, ALU.add, r=['ost'], w=['ost'])
            P.act(ost[:, :, 2:3], ost[:, :, 1:2], AF.Sqrt, r=['ost'], w=['ost'])
            P.op('dve', lambda e: e.reciprocal(out=ost[:, :, 3:4], in_=ost[:, :, 2:3]), ['ost'], ['ost'])
            og, ogr = oTg.next()
            for h in HS:
                P.ts('pool', Wk["on"][h][:], Wk["o"][h][:], ost[:, h, 3:4], None, ALU.mult, r=[rs("o", h), 'ost'], w=[rs("on", h)])
            stage(lambda h, pq, br: P.tr(pq, Wk["on"][h][:], identf, r=[rs("on", h), 'cs'], w=[br]),
                  [lambda h, pq, br: P.stt('dve', og[:, h, :], pq, ong[:, 0:1], z[:, h, :], ALU.mult, ALU.mult,
                                           r=[br, 'ong', zr], w=[ogr])])
            if STG < 8:
                continue
            ho, hor = ho_.next()
            for half in range(2):
                pb, pbr = pbig.next()
                for h in HS:
                    P.mm(pb[:], og[:, h, :], wout[:, h, half * 512:(half + 1) * 512], start=(h == 0), stop=(h == HS[-1]),
                         r=[ogr, 'wout'], w=[pbr])
                P.tt('dve', ho[:, half * 512:(half + 1) * 512], pb[:], xx[:, half * 512:(half + 1) * 512], ALU.add,
                     r=[pbr, xr], w=[hor])
            P.dma('sp', h1_s[t * 128:(t + 1) * 128, :], ho[:], r=[hor], key=hor)
        P.run()


def phase_mlp(nc, name, h_in, h_out, gam_mlp, gam_final, cst, wup_l, wdn_l):
    with ExitStack() as es:
        sb = lambda n, s, d: _sb(nc, es, n, s, d)
        Wu = sb("Wu", [128, 32, 8, 128], BF16)
        Wd = sb("Wd", [128, 32, 1024], BF16)
        gamt = sb("gamt", [128, D], F32)
        gamf = sb("gamf", [128, D], F32) if gam_final is not None else None
        idf = sb("idf", [128, 128], F32)
        identb = sb("identb", [128, 128], BF16)
        a1T = sb("a1T", [128, 32, 256], BF16)
        hmid = Rot([(sb(f"hmid{i}", [128, D], F32), f"hmid{i}") for i in range(4)])
        hn = Rot([(sb(f"hn{i}", [128, D], BF16), f"hn{i}") for i in range(2)])
        stat = Rot([(sb(f"stat{i}", [128, 4], F32), f"stat{i}") for i in range(2)])
        fstat = Rot([(sb(f"fstat{i}", [128, 4], F32), f"fstat{i}") for i in range(2)])
        junk = sb("junk", [128, D], BF16)
        hnT = Rot([(sb(f"hnT{i}", [128, 8, 256], BF16), f"hnT{i}") for i in range(2)])
        rl = Rot([(sb(f"rl{i}", [128, 256], F32), f"rl{i}") for i in range(3)])
        ost = Rot([(sb(f"ost{i}", [128, D], F32), f"ost{i}") for i in range(2)])
        ptr = Rot([(_ps(nc, es, f"ptr{i}", [128, 1024], BF16), f"PS:ptr{i}") for i in range(2)])
        pup = Rot([(_ps(nc, es, f"pup{i}", [128, 512], F32), f"PS:pup{i}") for i in range(3)])
        pdn = Rot([(_ps(nc, es, f"pdn{i}", [128, 512], F32), f"PS:pdn{i}") for i in range(3)])

        P = Phase(nc, name)
        P.dma('sp', idf[:], cst[:, C_ID:C_ID + 128], w=['idf'], key='idf')
        P.dma('sp', gamt[:], gam_mlp.partition_broadcast(128), w=['gam'], key='gam')
        if gamf is not None:
            P.dma('sp', gamf[:], gam_final.partition_broadcast(128), w=['gamf'], key='gamf')
        for i in range(8):
            P.dma('pool', Wu[:, 4 * i:4 * i + 4], wup_l[:, 4 * i:4 * i + 4], w=[f'Wu{i}'], key=f'Wu{i}')
        for i in range(8):
            P.dma('pool', Wd[:, 4 * i:4 * i + 4], wdn_l[:, 4 * i:4 * i + 4], w=[f'Wd{i}'], key=f'Wd{i}')
        P.copy('dve', identb[:], idf[:], r=['idf'], w=['identb'])
        for tb in range(16):
            hT, hTr = hnT.next()
            hm = []
            for j in range(2):
                t = tb * 2 + j
                xm = hmid.next()
                hm.append(xm)
                emit_norm_T(P, h_in[t * 128:(t + 1) * 128, :], gamt, xm, hn.next(), stat.next(), junk,
                            ptr.next(), hT[:, :, j * 128:(j + 1) * 128], identb, name, 'act' if j % 2 else 'dve',
                            f"{hTr}_{j}")
            hres = [f"{hTr}_{j}" for j in range(2)]
            for f in range(32):
                pu, pur = pup.next()
                for c in range(8):
                    P.mm(pu[:, 0:256], Wu[:, f, c, :], hT[:, c, :], start=(c == 0), stop=(c == 7),
                         r=hres + [f'Wu{f // 4}'], w=[pur])
                r_, rr = rl.next()
                P.act(r_[:], pu[:, 0:256], AF.Relu, r=[pur], w=[rr])
                P.tt('pool', a1T[:, f, :], r_[:], r_[:], ALU.mult, r=[rr], w=[f'a1T{f}'])
            ares = [f'a1T{f}' for f in range(32)]
            for j in range(2):
                t = tb * 2 + j
                xm, xmr = hm[j]
                o_, orr = ost.next()
                for half in range(2):
                    pd, pdr = pdn.next()
                    for f in range(32):
                        P.mm(pd[:], a1T[:, f, j * 128:(j + 1) * 128], Wd[:, f, half * 512:(half + 1) * 512],
                             start=(f == 0), stop=(f == 31), r=[f'a1T{f}', f'Wd{f // 4}'], w=[pdr])
                    P.tt('dve', o_[:, half * 512:(half + 1) * 512], pd[:], xm[:, half * 512:(half + 1) * 512], ALU.add,
                         r=[pdr, xmr], w=[orr])
                if gamf is not None:
                    fs, fsr = fstat.next()
                    P.act(junk[:], o_[:], AF.Square, accum=fs[:, 0:1], r=[orr], w=[fsr])
                    P.ts('dve', fs[:, 1:2], fs[:, 0:1], 1.0 / D, EPS, ALU.mult, ALU.add, r=[fsr], w=[fsr])
                    P.act(fs[:, 2:3], fs[:, 1:2], AF.Sqrt, r=[fsr], w=[fsr])
                    P.op('dve', (lambda fs=fs: (lambda e: e.reciprocal(out=fs[:, 3:4], in_=fs[:, 2:3])))(), [fsr], [fsr])
                    P.stt('dve', o_[:], o_[:], fs[:, 3:4], gamf[:], ALU.mult, ALU.mult, r=[orr, fsr, 'gamf'], w=[orr])
                P.dma('sp', h_out[t * 128:(t + 1) * 128, :], o_[:], r=[orr], key=orr)
        P.run()


def phase_moba_proj(nc, h_in, gam, cst, mb_wqk, mb_wv, mq_s, mk_s, mv_s, msel_s):
    with ExitStack() as es:
        sb = lambda n, s, d: _sb(nc, es, n, s, d)
        W = sb("W", [128, 16, 8, 128], BF16)
        Wv = sb("Wv", [128, 8, 1024], BF16)
        gamt = sb("gamt", [128, D], F32)
        cs = sb("cs", [128, NCST], F32)
        identb = sb("identb", [128, 128], BF16)
        kms = sb("kms", [128, 8, 16], F32)
        xin = Rot([(sb(f"xin{i}", [128, D], F32), f"xin{i}") for i in range(2)])
        hn = Rot([(sb(f"hn{i}", [128, D], BF16), f"hn{i}") for i in range(2)])
        stat = Rot([(sb(f"stat{i}", [128, 4], F32), f"stat{i}") for i in range(2)])
        junk = sb("junk", [128, D], BF16)
        hnT = Rot([(sb(f"hnT{i}", [128, 8, 512], BF16), f"hnT{i}") for i in range(2)])
        kblk = Rot([(sb(f"kblk{i}", [128, 8, 512], BF16), f"kblk{i}") for i in range(2)])
        qblk = Rot([(sb(f"qblk{i}", [128, 8, 512], BF16), f"qblk{i}") for i in range(2)])
        q32 = Rot([(sb(f"q32{i}", [128, 512], F32), f"q32{i}") for i in range(2)])
        selb = Rot([(sb(f"selb{i}", [128, 4, 8, 16], F32), f"selb{i}") for i in range(2)])
        selTb = Rot([(sb(f"selTb{i}", [128, 512], BF16), f"selTb{i}") for i in range(2)])
        gm = Rot([(sb(f"gm{i}", [128, 16], F32), f"gm{i}") for i in range(4)])
        t8 = Rot([(sb(f"t8{i}", [128, 8], F32), f"t8{i}") for i in range(4)])
        vaug = Rot([(sb(f"vaug{i}", [128, 8, 129], BF16), f"vaug{i}") for i in range(2)])
        ptr = Rot([(_ps(nc, es, f"ptr{i}", [128, 1024], BF16), f"PS:ptr{i}") for i in range(2)])
        pmm = Rot([(_ps(nc, es, f"pmm{i}", [128, 512], F32), f"PS:pmm{i}") for i in range(3)])
        pgt = _ps(nc, es, "pgt", [128, 512], F32)
        pst = _ps(nc, es, "pst", [128, 512], F32)

        P = Phase(nc, "mp")
        P.dma('sp', cs[:], cst, w=['cs'], key='cs')
        P.dma('sp', gamt[:], gam[2].partition_broadcast(128), w=['gam'], key='gam')
        for i in range(4):
            P.dma('pool', W[:, 4 * i:4 * i + 4], mb_wqk[:, 4 * i:4 * i + 4], w=[f'W{i}'], key=f'W{i}')
        P.dma('pool', Wv[:], mb_wv, w=['Wv'], key='Wv')
        P.copy('dve', identb[:], cs[:, C_ID:C_ID + 128], r=['cs'], w=['identb'])
        P.memset('pool', kms[:], 0.0, w=['kms'])
        for i in range(2):
            va, var = vaug.next()
            P.memset('pool', va[:], 1.0, w=[var])
        identf = cs[:, C_ID:C_ID + 128]
        scale = 128.0 ** -0.5
        for tb in range(8):
            hT, hTr = hnT.next()
            for j in range(4):
                t = tb * 4 + j
                emit_norm_T(P, h_in[t * 128:(t + 1) * 128, :], gamt, xin.next(), hn.next(), stat.next(), junk,
                            ptr.next(), hT[:, :, j * 128:(j + 1) * 128], identb, "mp", 'act' if j % 2 else 'dve',
                            f"{hTr}_{j}")
            hres = [f"{hTr}_{j}" for j in range(4)]
            sl = slice(tb * 512, (tb + 1) * 512)
            kb, kbr = kblk.next()
            for h in range(8):
                pm, pmr = pmm.next()
                for c in range(8):
                    P.mm(pm[:], W[:, 8 + h, c, :], hT[:, c, :], start=(c == 0), stop=(c == 7),
                         r=hres + [f'W{(8 + h) // 4}'], w=[pmr])
                P.copy('act', kb[:, h, :], pm[:], r=[pmr], w=[kbr])
                P.op('dve', (lambda pm=pm, h=h, tb=tb: (lambda e: e.tensor_reduce(
                    out=kms[:, h, 2 * tb:2 * tb + 2], in_=pm[:].rearrange("p (a b) -> p a b", b=256),
                    axis=AX.X, op=ALU.add)))(), [pmr], ['kms'])
            P.dma('sp', mk_s[:, :, sl], kb[:], r=[kbr], key=kbr)
            qb_, qbr = qblk.next()
            sbt, sbr = selb.next()
            for h in range(8):
                pm, pmr = pmm.next()
                for c in range(8):
                    P.mm(pm[:], W[:, h, c, :], hT[:, c, :], start=(c == 0), stop=(c == 7),
                         r=hres + [f'W{h // 4}'], w=[pmr])
                qf, qfr = q32.next()
                P.copy('act', qf[:], pm[:], r=[pmr], w=[qfr])
                P.ts('dve', qb_[:, h, :], pm[:], scale, None, ALU.mult, r=[pmr], w=[qbr])
                for j in range(4):
                    t = tb * 4 + j
                    qblock = t // 2
                    P.mm(pgt[:, (h * 4 + j) * 16:(h * 4 + j) * 16 + 16], qf[:, j * 128:(j + 1) * 128], kms[:, h, :],
                         r=[qfr, 'kms'], w=['PS:pgt'])
                for j in range(4):
                    t = tb * 4 + j
                    qblock = t // 2
                    g_, gr_ = gm.next()
                    t8_, t8r = t8.next()
                    P.tt('dve', g_[:], pgt[:, (h * 4 + j) * 16:(h * 4 + j) * 16 + 16],
                         cs[:, C_PAST + qblock * 16:C_PAST + qblock * 16 + 16], ALU.add, r=['PS:pgt', 'cs'], w=[gr_])
                    P.op('dve', (lambda t8_=t8_, g_=g_: (lambda e: e.max(out=t8_[:], in_=g_[:])))(), [gr_], [t8r])
                    P.ts('dve', g_[:], g_[:], t8_[:, 2:3], None, ALU.is_ge, r=[gr_, t8r], w=[gr_])
                    P.ts('dve', sbt[:, j, h, :], g_[:], 1.0, -NEG, ALU.subtract, ALU.mult, r=[gr_], w=[sbr])
            P.dma('sp', mq_s[:, :, sl], qb_[:], r=[qbr], key=qbr)
            stb, stbr = selTb.next()
            for j in range(4):
                P.tr(pst[:, j * 128:(j + 1) * 128], sbt[:, j].rearrange("p h n -> p (h n)"), identf, r=[sbr, 'cs'], w=['PS:pst'])
            P.copy('act', stb[:], pst[:], r=['PS:pst'], w=[stbr])
            P.dma('sp', msel_s[:, :, sl].rearrange("h n t -> (h n) t"), stb[:], r=[stbr], key=stbr)
            for j in range(4):
                t = tb * 4 + j
                va, var = vaug.next()
                for half in range(2):
                    pm, pmr = pmm.next()
                    for c in range(8):
                        P.mm(pm[:], hT[:, c, j * 128:(j + 1) * 128], Wv[:, c, half * 512:(half + 1) * 512],
                             start=(c == 0), stop=(c == 7), r=[hres[j], 'Wv'], w=[pmr])
                    P.copy('act' if half else 'dve', va[:, half * 4:half * 4 + 4, 0:128],
                           pm[:].rearrange("p (h d) -> p h d", h=4), r=[pmr], w=[var])
                P.dma('sp', mv_s[t * 128:(t + 1) * 128], va[:], r=[var], key=var)
        P.run()


def phase_moba_attn(nc, cst, sel, mq_s, mk_s, mv_s, msel_s, o2T_s):
    with ExitStack() as es:
        sb = lambda n, s, d: _sb(nc, es, n, s, d)
        cs = sb("cs", [128, NCST], F32)
        selt = sb("selt", [16, 2048], F32)
        selb16 = sb("selb16", [16, 2048], BF16)
        identb = sb("identb", [128, 128], BF16)
        causb = sb("causb", [128, 128], BF16)
        qh = Rot([(sb(f"qh{i}", [128, T], BF16), f"qh{i}") for i in range(2)])
        kh = Rot([(sb(f"kh{i}", [128, T], BF16), f"kh{i}") for i in range(2)])
        vh = Rot([(sb(f"vh{i}", [128, NT, 129], BF16), f"vh{i}") for i in range(2)])
        sh = Rot([(sb(f"sh{i}", [16, T], BF16), f"sh{i}") for i in range(2)])
        oTh = Rot([(sb(f"oTh{i}", [128, T], BF16), f"oTh{i}") for i in range(2)])
        pT = Rot([(sb(f"pT{i}", [128, 128], BF16), f"pT{i}") for i in range(10)])
        rden = Rot([(sb(f"rden{i}", [128, 1], F32), f"rden{i}") for i in range(3)])
        onb = Rot([(sb(f"onb{i}", [128, 128], BF16), f"onb{i}") for i in range(3)])
        banks = [_ps(nc, es, f"bk{i}", [128, 512], F32) for i in range(6)]
        sbanks = Rot([(banks[i], f"PS:sbk{i}") for i in range(4)])
        pacc = Rot([(banks[4 + i], f"PS:pacc{i}") for i in range(2)])
        ptr = Rot([(_ps(nc, es, f"ptr{i}", [128, 1024], BF16), f"PS:ptr{i}") for i in range(2)])

        P = Phase(nc, "ma")
        P.dma('sp', cs[:], cst, w=['cs'], key='cs')
        P.dma('sp', selt[:], sel, w=['selt'], key='selt')
        P.copy('dve', identb[:], cs[:, C_ID:C_ID + 128], r=['cs'], w=['identb'])
        P.copy('dve', causb[:], cs[:, C_MU:C_MU + 128], r=['cs'], w=['causb'])
        P.copy('dve', selb16[:], selt[:], r=['selt'], w=['selb16'])

        def loads(h):
            q, qr = qh.next(); k, kr = kh.next(); v, vr = vh.next(); s, sr = sh.next()
            P.dma('sp', q[:], mq_s[:, h, :], w=[qr], key=qr)
            P.dma('sp', k[:], mk_s[:, h, :], w=[kr], key=kr)
            for i in range(4):
                P.dma('sp', v[:, i * 8:(i + 1) * 8, :],
                      mv_s[i * 1024:(i + 1) * 1024, h, :].rearrange("(t p) d -> p t d", p=128),
                      w=[vr + f"_{i}"], key=vr + f"_{i}")
            P.dma('sp', s[:], msel_s[h], w=[sr], key=sr)
            return (q, qr, k, kr, v, vr, s, sr)

        nxt = loads(0)
        import os
        NHL = int(os.environ.get('MA_HEADS', 8))
        for h in range(NHL):
            q, qr, k, kr, v, vr, s, sr = nxt
            if h + 1 < NHL:
                nxt = loads(h + 1)
            oT, oTr = oTh.next()
            slope_h = 2.0 ** -(h + 1)
            items = []
            for qt in range(NT):
                allk = [kt for kt in range(qt + 1) if kt >= qt - 1 or slope_h * ((qt - kt - 1) * 128 + 1) <= 200.0]
                for g0 in range(0, len(allk), 4):
                    items.append(dict(qt=qt, kts=allk[g0:g0 + 4], first=allk[0], last=(g0 + 4 >= len(allk))))
            accs = {}
            pending = []

            def emit_qk(it):
                qt = it['qt']
                qblock = qt // 2
                qs = slice(qt * 128, (qt + 1) * 128)
                sbk, sbr_ = sbanks.next()
                it['sbk'] = (sbk, sbr_)
                for i, kt in enumerate(it['kts']):
                    b = kt // 2
                    ks = slice(kt * 128, (kt + 1) * 128)
                    p1 = sbk[:, i * 128:(i + 1) * 128]
                    nomask = (b == qblock and kt != qt)
                    P.mm(p1, k[:, ks], q[:, qs], start=True, stop=nomask, r=[kr, qr], w=[sbr_])
                    if b < qblock:
                        P.mm(p1, selb16[:, b * 128:(b + 1) * 128], s[:, qs], start=False, stop=True,
                             r=['selb16', sr], w=[sbr_])
                    elif kt == qt:
                        P.mm(p1, identb[:], causb[:], start=False, stop=True, r=['identb', 'causb'], w=[sbr_])

            def emit_exp_pv(it):
                qt = it['qt']
                qs = slice(qt * 128, (qt + 1) * 128)
                if qt not in accs:
                    accs[qt] = pacc.next()
                pa, par = accs[qt]
                sbk, sbr_ = it['sbk']
                for i, kt in enumerate(it['kts']):
                    p1 = sbk[:, i * 128:(i + 1) * 128]
                    pt, ptr_ = pT.next()
                    P.act(pt[:], p1, AF.Exp, bias=cs[:, C_ALI + h * 32 + (qt - kt):C_ALI + h * 32 + (qt - kt) + 1],
                          r=[sbr_, 'cs'], w=[ptr_])
                    P.mm(pa[:, 0:129], pt[:], v[:, kt, :], start=(kt == it['first']), stop=(kt == qt),
                         r=[ptr_, vr + f"_{kt // 8}"], w=[par])
                if it['last']:
                    rd, rdr = rden.next()
                    ob, obr = onb.next()
                    P.op('dve', (lambda rd=rd, pa=pa: (lambda e: e.reciprocal(out=rd[:], in_=pa[:, 128:129])))(), [par], [rdr])
                    P.ts('dve', ob[:], pa[:, 0:128], rd[:, 0:1], None, ALU.mult, r=[par, rdr], w=[obr])

                    def fin(ob=ob, obr=obr, qs=qs):
                        pt2, pt2r = ptr.next()
                        P.tr(pt2[:, 0:128], ob[:], identb[:], r=[obr, 'identb'], w=[pt2r])
                        P.copy('dve', oT[:, qs], pt2[:, 0:128], r=[pt2r], w=[oTr])
                    pending.append(fin)

            emit_qk(items[0])
            for i, it in enumerate(items):
                if i + 1 < len(items):
                    emit_qk(items[i + 1])
                while pending:
                    pending.pop(0)()
                emit_exp_pv(it)
            while pending:
                pending.pop(0)()
            P.dma('sp', o2T_s[:, h, :], oT[:], r=[oTr], key=oTr)
        P.run()


def phase_outproj(nc, oT_s, wout_d, h_in, h_out):
    with ExitStack() as es:
        sb = lambda n, s, d: _sb(nc, es, n, s, d)
        wout = sb("wout", [128, 8, 1024], BF16)
        ot = Rot([(sb(f"ot{i}", [128, 8, 128], BF16), f"ot{i}") for i in range(3)])
        xt = Rot([(sb(f"xt{i}", [128, D], F32), f"xt{i}") for i in range(3)])
        ho = Rot([(sb(f"ho{i}", [128, D], F32), f"ho{i}") for i in range(2)])
        pb = Rot([(_ps(nc, es, f"pb{i}", [128, 512], F32), f"PS:pb{i}") for i in range(4)])
        P = Phase(nc, "op")
        P.dma('pool', wout[:], wout_d, w=['wout'], key='wout')
        for t in range(NT):
            sl = slice(t * 128, (t + 1) * 128)
            o, orr = ot.next(); xx, xr = xt.next(); hh, hr = ho.next()
            P.dma('sp', o[:], oT_s[:, :, sl], w=[orr], key=orr)
            P.dma('sp', xx[:], h_in[sl, :], w=[xr], key=xr)
            for half in range(2):
                p, pr = pb.next()
                for h in range(8):
                    P.mm(p[:], o[:, h, :], wout[:, h, half * 512:(half + 1) * 512], start=(h == 0), stop=(h == 7),
                         r=[orr, 'wout'], w=[pr])
                P.tt('dve', hh[:, half * 512:(half + 1) * 512], p[:], xx[:, half * 512:(half + 1) * 512], ALU.add,
                     r=[pr, xr], w=[hr])
            P.dma('sp', h_out[sl, :], hh[:], r=[hr], key=hr)
        P.run()


def make_consts():
    c = np.zeros((128, NCST), np.float32)
    j = np.arange(128)[:, None]
    i = np.arange(128)[None, :]
    c[:, C_ID:C_ID + 128] = (i == j)
    c[:, C_MU:C_MU + 128] = np.where(i >= j, 0.0, NEG)
    c[:, C_MSU:C_MSU + 128] = np.where(i > j, 0.0, NEG)
    c[:, C_TRI:C_TRI + 128] = (j <= i)
    c[:, C_ONE:C_ONE + 128] = 1.0
    p = np.arange(128, dtype=np.float64)
    for h in range(8):
        slope = 2.0 ** (-8.0 * (h + 1) / 8)
        for dlt in range(32):
            c[:, C_ALI + h * 32 + dlt] = slope * (p - dlt * 128.0)
    for qb in range(16):
        for n in range(16):
            c[:, C_PAST + qb * 16 + n] = 0.0 if n < qb else -1e30
    s = np.zeros((16, 2048), np.float32)
    for b in range(16):
        s[b, b * 128:(b + 1) * 128] = 1.0
    return c, s


def prep_shared(inp):
    f = lambda a: np.ascontiguousarray(a, dtype=np.float32)
    cst, sel = make_consts()
    w = inp["dn_w_in"][0]
    mw = inp["mb_w_in"][0]
    sh = {
        "gam": f(np.concatenate([inp["mix_norm_g"][0:1], inp["mlp_norm_g"][0:1], inp["mix_norm_g"][1:2],
                                 inp["mlp_norm_g"][1:2], inp["final_norm_g"][None, :]], axis=0)),
        "cst": cst, "sel": sel,
        "dn_win": f(w[:, :4096].reshape(8, 128, 32, 128).transpose(1, 2, 0, 3)),
        "dn_wg": f(w[:, 4096:].reshape(8, 128, 16).transpose(1, 0, 2)),
        "dn_cw": f(inp["dn_conv_w"][0].reshape(4, 24, 128).transpose(2, 1, 0)),
        "dn_vec": f(np.concatenate([inp["dn_a_log"][0], inp["dn_dt_bias"][0]])),
        "dn_ong": f(inp["dn_out_norm_g"][0].reshape(128, 1)),
        "dn_wout": f(inp["dn_w_out"][0].reshape(8, 128, 1024).transpose(1, 0, 2)),
        "mb_wqk": f(mw[:, :2048].reshape(8, 128, 16, 128).transpose(1, 2, 0, 3)),
        "mb_wv": f(mw[:, 2048:].reshape(8, 128, 1024).transpose(1, 0, 2)),
        "mb_wout": f(inp["mb_w_out"][0].reshape(8, 128, 1024).transpose(1, 0, 2)),
        "wup": f(inp["mlp_w_up"].reshape(2, 8, 128, 32, 128).transpose(0, 2, 3, 1, 4)),
        "wdn": f(inp["mlp_w_down"].reshape(2, 32, 128, 1024).transpose(0, 2, 1, 3)),
    }
    return sh


_NC_CACHE = {}


def kernel(**inputs):
    inp = {k: np.asarray(v) for k, v in inputs.items()}
    sh = prep_shared(inp)
    if "nc" not in _NC_CACHE:
        _NC_CACHE["nc"] = build()
    nc = _NC_CACHE["nc"]
    x = np.ascontiguousarray(inp["x"], dtype=np.float32)
    in_maps = [dict(sh, x=x[b]) for b in range(8)]
    res = run_bass_kernel_spmd(nc, in_maps, core_ids=list(range(8)))
    return np.stack([np.asarray(r["out"]) for r in res.results], axis=0).astype(np.float32)
```
